# Optimizing a Trainium2 kernel written in Bass

```python
import math
import jax, jax.numpy as jnp
from jax import lax
import numpy as np

D_MODEL = 1024
BATCH = 8
SEQ = 2048
DEPTH = 2
DEC_BATCH = 128
DEC_SEQ = 8
PAST_LEN = 16384
PAGE_SIZE = 128

GLA_H = 4
GLA_DK = 32
GLA_DV = 64
GLA_WIDTH = GLA_H * GLA_DV
GLA_GATE_RANK = 16
GLA_GATE_TEMP = 16.0
GLA_CHUNK = 16
RET_H = 4
RET_DK = 64
RET_DV = 64
RET_WIDTH = RET_H * RET_DV
RET_CHUNK = 64
ROPE_BASE = 10000.0
SSD_H = 8
SSD_P = 64
SSD_WIDTH = SSD_H * SSD_P
SSD_G = 2
SSD_N = 64
SSD_CONV_W = 4
SSD_CONV_DIM = SSD_WIDTH + 2 * SSD_G * SSD_N
SSD_CHUNK = 64
D_MIX = GLA_WIDTH + RET_WIDTH + SSD_WIDTH
MOE_GROUPS = 4
MOE_PER_GROUP = 4
MOE_EXPERTS = MOE_GROUPS * MOE_PER_GROUP
MOE_TOPK = 2
MOE_FF = 256
ALPHA = (2 * DEPTH) ** 0.25
BETA = (8 * DEPTH) ** -0.25
EPS = 1e-5
IN_WIDTHS = (GLA_H * GLA_DK, GLA_H * GLA_DK, GLA_WIDTH, GLA_GATE_RANK, GLA_WIDTH,
             RET_H * RET_DK, RET_H * RET_DK, RET_WIDTH, RET_WIDTH,
             SSD_WIDTH, SSD_CONV_DIM, SSD_H)
N_IN = sum(IN_WIDTHS)

kernel_name = 'hymba_gla_ret_ssd_hmoe_step'

F32 = jnp.float32


def layer_norm(x, g, b):
    xf = x.astype(F32)
    mu = jnp.mean(xf, -1, keepdims=True)
    var = jnp.mean(jnp.square(xf - mu), -1, keepdims=True)
    return (xf - mu) * lax.rsqrt(var + EPS) * g.astype(F32) + b.astype(F32)


def rms_norm(x, g):
    xf = x.astype(F32)
    return xf * lax.rsqrt(jnp.mean(jnp.square(xf), -1, keepdims=True) + EPS) * g.astype(F32)


def head_group_norm(x, g):
    xf = x.astype(F32)
    mu = jnp.mean(xf, -1, keepdims=True)
    var = jnp.mean(jnp.square(xf - mu), -1, keepdims=True)
    y = (xf - mu) * lax.rsqrt(var + EPS)
    return y.reshape(x.shape[0], x.shape[1], -1) * g.astype(F32)


def rope(x, pos):
    half = x.shape[-1] // 2
    inv = ROPE_BASE ** (-jnp.arange(half, dtype=F32) / half)
    ang = pos.astype(F32)[:, None] * inv[None, :]
    cos = jnp.cos(ang)[None, :, None, :]
    sin = jnp.sin(ang)[None, :, None, :]
    x1 = x[..., :half].astype(F32)
    x2 = x[..., half:].astype(F32)
    return jnp.concatenate([x1 * cos - x2 * sin, x1 * sin + x2 * cos], -1)


def chunked_linear_recurrence(q, k, v, log_a, s0, chunk):
    B, T, H, K = q.shape
    V = v.shape[-1]
    c = math.gcd(chunk, T)
    n = T // c
    q = q.astype(F32).reshape(B, n, c, H, K)
    k = k.astype(F32).reshape(B, n, c, H, K)
    v = v.astype(F32).reshape(B, n, c, H, V)
    g = jnp.cumsum(log_a.astype(F32).reshape(B, n, c, H, -1), axis=2)
    causal = jnp.tril(jnp.ones((c, c), dtype=bool))[None, None, :, :, None, None]
    diff = g[:, :, :, None] - g[:, :, None, :]
    decay = jnp.exp(jnp.where(causal, diff, -jnp.inf))
    if g.shape[-1] == 1:
        scores = jnp.einsum('bnihk,bnjhk->bnijh', q, k) * decay[..., 0]
    else:
        scores = jnp.einsum('bnihk,bnjhk,bnijhk->bnijh', q, k, decay)
    o_intra = jnp.einsum('bnijh,bnjhv->bnihv', scores, v)
    g_last = g[:, :, -1]
    u = jnp.einsum('bnjhk,bnjhv->bnhkv', k * jnp.exp(g_last[:, :, None] - g), v)
    a_chunk = jnp.exp(g_last)

    def step(s, inp):
        a_c, u_c = inp
        return a_c[..., None] * s + u_c, s

    s_final, s_starts = lax.scan(step, s0.astype(F32),
                                 (jnp.moveaxis(a_chunk, 1, 0), jnp.moveaxis(u, 1, 0)))
    s_starts = jnp.moveaxis(s_starts, 0, 1)
    o_inter = jnp.einsum('bnihk,bnhkv->bnihv', q * jnp.exp(g), s_starts)
    return (o_intra + o_inter).reshape(B, T, H, V), s_final


def causal_dwconv(x, prev, w, b):
    T = x.shape[1]
    xx = jnp.concatenate([prev.astype(x.dtype), x], axis=1)
    out = b + sum(w[i] * xx[:, i:i + T] for i in range(SSD_CONV_W))
    return out, xx[:, -(SSD_CONV_W - 1):]


def mixer(h, pos0, s_gla, s_ret, s_ssd, s_conv, l, p):
    B, T, _ = h.shape
    splits = [int(s) for s in np.cumsum(IN_WIDTHS)[:-1]]
    proj = h @ p['w_in'][l]
    gq, gk, gv, ga, gr, rq, rk, rv, rg, sz, sxbc, sdt = jnp.split(proj, splits, axis=-1)

    q = gq.reshape(B, T, GLA_H, GLA_DK) * GLA_DK ** -0.5
    k = gk.reshape(B, T, GLA_H, GLA_DK)
    v = gv.reshape(B, T, GLA_H, GLA_DV)
    gate = (ga @ p['gla_w_gate'][l] + p['gla_b_gate'][l]).astype(F32)
    log_a = (jax.nn.log_sigmoid(gate) / GLA_GATE_TEMP).reshape(B, T, GLA_H, GLA_DK)
    o, s_gla_new = chunked_linear_recurrence(q, k, v, log_a, s_gla, GLA_CHUNK)
    o_gla = rms_norm(o, p['gla_norm'][l].reshape(GLA_H, GLA_DV)).reshape(B, T, GLA_WIDTH)
    o_gla = o_gla * jax.nn.silu(gr.astype(F32))

    pos = pos0 + jnp.arange(T, dtype=jnp.int32)
    q = rope(rq.reshape(B, T, RET_H, RET_DK), pos)
    k = rope(rk.reshape(B, T, RET_H, RET_DK), pos) * RET_DK ** -0.5
    v = rv.reshape(B, T, RET_H, RET_DV)
    log_gamma = jnp.log(1.0 - 2.0 ** (-5.0 - jnp.arange(RET_H, dtype=F32)))
    log_a = jnp.broadcast_to(log_gamma[None, None, :, None], (B, T, RET_H, 1))
    o, s_ret_new = chunked_linear_recurrence(q, k, v, log_a, s_ret, RET_CHUNK)
    o_ret = head_group_norm(o, p['ret_norm'][l]) * jax.nn.silu(rg.astype(F32))

    xbc, s_conv_new = causal_dwconv(sxbc, s_conv, p['ssd_conv_w'][l], p['ssd_conv_b'][l])
    xbc = jax.nn.silu(xbc.astype(F32))
    xs, bm, cm = jnp.split(xbc, [SSD_WIDTH, SSD_WIDTH + SSD_G * SSD_N], axis=-1)
    xs = xs.reshape(B, T, SSD_H, SSD_P)
    bm = jnp.repeat(bm.reshape(B, T, SSD_G, SSD_N), SSD_H // SSD_G, axis=2)
    cm = jnp.repeat(cm.reshape(B, T, SSD_G, SSD_N), SSD_H // SSD_G, axis=2)
    dt = jax.nn.softplus(sdt.astype(F32) + p['ssd_dt_bias'][l].astype(F32))
    a = -jnp.exp(p['ssd_a_log'][l].astype(F32))
    o, s_ssd_new = chunked_linear_recurrence(cm, bm, xs * dt[..., None],
                                             (dt * a)[..., None], s_ssd, SSD_CHUNK)
    y = o + p['ssd_d'][l].astype(F32)[:, None] * xs
    o_ssd = rms_norm(y.reshape(B, T, SSD_WIDTH) * jax.nn.silu(sz.astype(F32)), p['ssd_norm'][l])

    merged = jnp.concatenate([o_gla, o_ret, o_ssd], axis=-1).astype(h.dtype)
    out = merged @ p['w_out'][l]
    return out, (s_gla_new, s_ret_new, s_ssd_new, s_conv_new)


def hier_moe(h, l, p):
    B, T, D = h.shape
    t = h.reshape(B * T, D)
    p_group = jax.nn.softmax((t @ p['moe_w_group'][l] + p['moe_b_group'][l]).astype(F32), -1)
    g_sel = jnp.argmax(p_group, -1)
    g_gate = jnp.take_along_axis(p_group, g_sel[:, None], -1)
    e_logits = (t @ p['moe_w_expert'][l] + p['moe_b_expert'][l]).astype(F32)
    e_logits = e_logits.reshape(-1, MOE_GROUPS, MOE_PER_GROUP)
    e_in = jnp.take_along_axis(e_logits, g_sel[:, None, None], 1)[:, 0]
    top_v, top_i = lax.top_k(e_in, MOE_TOPK)
    w_top = jax.nn.softmax(top_v, -1) * g_gate
    expert_idx = g_sel[:, None] * MOE_PER_GROUP + top_i
    combine = jnp.sum(jax.nn.one_hot(expert_idx, MOE_EXPERTS, dtype=F32) * w_top[..., None], 1)
    hid = jax.nn.silu(jnp.einsum('nd,edf->nef', t, p['moe_w1'][l])) * \
        jnp.einsum('nd,edf->nef', t, p['moe_w3'][l])
    y = jnp.einsum('nef,efd->nd', hid * combine[..., None].astype(hid.dtype), p['moe_w2'][l])
    return y.reshape(B, T, D)


def trunk(x, c, pos0, s_gla, s_ret, s_ssd, s_conv, p):
    new_gla, new_ret, new_ssd, new_conv = [], [], [], []
    for l in range(DEPTH):
        mod = jax.nn.silu(c) @ p['w_ada'][l] + p['b_ada'][l]
        sh1, sc1, g1, sh2, sc2, g2 = jnp.split(mod[:, None, :], 6, axis=-1)
        h = x * (1.0 + sc1) + sh1
        mix, st = mixer(h, pos0, s_gla[l], s_ret[l], s_ssd[l], s_conv[l], l, p)
        x = layer_norm(ALPHA * x + g1 * mix, p['ln1_g'][l], p['ln1_b'][l]).astype(x.dtype)
        h = x * (1.0 + sc2) + sh2
        x = layer_norm(ALPHA * x + g2 * hier_moe(h, l, p), p['ln2_g'][l], p['ln2_b'][l]).astype(x.dtype)
        new_gla.append(st[0])
        new_ret.append(st[1])
        new_ssd.append(st[2])
        new_conv.append(st[3])
    return x, jnp.stack(new_gla), jnp.stack(new_ret), jnp.stack(new_ssd), jnp.stack(new_conv)


def setup_inputs(seed: int = 0) -> dict:
    key = jax.random.key(seed)
    keys = iter(jax.random.split(key, 64))
    nrm = lambda shape, s=1.0: jax.random.normal(next(keys), shape, F32) * s
    D = D_MODEL
    value_cols = (2, 7, 10)
    col_scale = jnp.concatenate([
        jnp.full((w,), BETA if i in value_cols else 1.0, F32) for i, w in enumerate(IN_WIDTHS)])
    off = sum(IN_WIDTHS[:10])
    col_scale = col_scale.at[off + SSD_WIDTH:off + SSD_CONV_DIM].set(1.0)
    u = jax.random.uniform(next(keys), (DEPTH, SSD_H), F32)
    dt0 = jnp.exp(u * (math.log(0.1) - math.log(0.001)) + math.log(0.001))
    inp = {
        'x_prompt': nrm((BATCH, SEQ, D)),
        'x_sample': nrm((DEC_BATCH, DEC_SEQ, D)),
        'c_prompt': nrm((BATCH, D)),
        'c_sample': nrm((DEC_BATCH, D)),
        'state_gla': nrm((DEPTH, DEC_BATCH, GLA_H, GLA_DK, GLA_DV), 0.5),
        'state_ret': nrm((DEPTH, DEC_BATCH, RET_H, RET_DK, RET_DV), 0.5),
        'state_ssd': nrm((DEPTH, DEC_BATCH, SSD_H, SSD_N, SSD_P), 0.5),
        'state_conv': nrm((DEPTH, DEC_BATCH, SSD_CONV_W - 1, SSD_CONV_DIM)),
        'w_ada': nrm((DEPTH, D, 6 * D), D ** -0.5),
        'b_ada': nrm((DEPTH, 6 * D), 0.02),
        'w_in': nrm((DEPTH, D, N_IN), D ** -0.5) * col_scale,
        'gla_w_gate': nrm((DEPTH, GLA_GATE_RANK, GLA_H * GLA_DK), GLA_GATE_RANK ** -0.5),
        'gla_b_gate': nrm((DEPTH, GLA_H * GLA_DK), 0.5),
        'gla_norm': 1.0 + nrm((DEPTH, GLA_WIDTH), 0.02),
        'ret_norm': 1.0 + nrm((DEPTH, RET_WIDTH), 0.02),
        'ssd_conv_w': nrm((DEPTH, SSD_CONV_W, SSD_CONV_DIM), SSD_CONV_W ** -0.5),
        'ssd_conv_b': nrm((DEPTH, SSD_CONV_DIM), 0.01),
        'ssd_dt_bias': dt0 + jnp.log(-jnp.expm1(-dt0)),
        'ssd_a_log': jnp.log(jax.random.uniform(next(keys), (DEPTH, SSD_H), F32, 1.0, 16.0)),
        'ssd_d': 1.0 + nrm((DEPTH, SSD_H), 0.1),
        'ssd_norm': 1.0 + nrm((DEPTH, SSD_WIDTH), 0.02),
        'w_out': nrm((DEPTH, D_MIX, D), D_MIX ** -0.5 * BETA),
        'ln1_g': 1.0 + nrm((DEPTH, D), 0.02),
        'ln1_b': nrm((DEPTH, D), 0.02),
        'moe_w_group': nrm((DEPTH, D, MOE_GROUPS), D ** -0.5),
        'moe_b_group': nrm((DEPTH, MOE_GROUPS), 0.01),
        'moe_w_expert': nrm((DEPTH, D, MOE_EXPERTS), D ** -0.5),
        'moe_b_expert': nrm((DEPTH, MOE_EXPERTS), 0.01),
        'moe_w1': nrm((DEPTH, MOE_EXPERTS, D, MOE_FF), D ** -0.5 * BETA),
        'moe_w3': nrm((DEPTH, MOE_EXPERTS, D, MOE_FF), D ** -0.5 * BETA),
        'moe_w2': nrm((DEPTH, MOE_EXPERTS, MOE_FF, D), MOE_FF ** -0.5 * BETA),
        'ln2_g': 1.0 + nrm((DEPTH, D), 0.02),
        'ln2_b': nrm((DEPTH, D), 0.02),
    }
    return inp


def reference(x_prompt, x_sample, c_prompt, c_sample, state_gla, state_ret, state_ssd, state_conv,
              w_ada, b_ada, w_in, gla_w_gate, gla_b_gate, gla_norm, ret_norm, ssd_conv_w, ssd_conv_b,
              ssd_dt_bias, ssd_a_log, ssd_d, ssd_norm, w_out, ln1_g, ln1_b, moe_w_group, moe_b_group,
              moe_w_expert, moe_b_expert, moe_w1, moe_w3, moe_w2, ln2_g, ln2_b):
    p = {'w_ada': w_ada, 'b_ada': b_ada, 'w_in': w_in, 'gla_w_gate': gla_w_gate,
         'gla_b_gate': gla_b_gate, 'gla_norm': gla_norm, 'ret_norm': ret_norm,
         'ssd_conv_w': ssd_conv_w, 'ssd_conv_b': ssd_conv_b, 'ssd_dt_bias': ssd_dt_bias,
         'ssd_a_log': ssd_a_log, 'ssd_d': ssd_d, 'ssd_norm': ssd_norm, 'w_out': w_out,
         'ln1_g': ln1_g, 'ln1_b': ln1_b, 'moe_w_group': moe_w_group, 'moe_b_group': moe_b_group,
         'moe_w_expert': moe_w_expert, 'moe_b_expert': moe_b_expert, 'moe_w1': moe_w1,
         'moe_w3': moe_w3, 'moe_w2': moe_w2, 'ln2_g': ln2_g, 'ln2_b': ln2_b}
    B = x_prompt.shape[0]
    dt = x_prompt.dtype
    z_gla = jnp.zeros((DEPTH, B, GLA_H, GLA_DK, GLA_DV), dt)
    z_ret = jnp.zeros((DEPTH, B, RET_H, RET_DK, RET_DV), dt)
    z_ssd = jnp.zeros((DEPTH, B, SSD_H, SSD_N, SSD_P), dt)
    z_conv = jnp.zeros((DEPTH, B, SSD_CONV_W - 1, SSD_CONV_DIM), dt)
    y_prompt, gla_p, ret_p, ssd_p, conv_p = trunk(x_prompt, c_prompt, 0, z_gla, z_ret, z_ssd, z_conv, p)
    y_sample, gla_s, ret_s, ssd_s, conv_s = trunk(x_sample, c_sample, PAST_LEN, state_gla, state_ret,
                                                  state_ssd, state_conv, p)
    return (y_prompt, y_sample, gla_p, ret_p, ssd_p, conv_p, gla_s, ret_s, ssd_s, conv_s)
```

```python
import os
import numpy as np
from contextlib import ExitStack
import concourse.bass as bass
import concourse.mybir as mybir
from concourse.bass_utils import run_bass_kernel_spmd

F32 = mybir.dt.float32
BF16 = mybir.dt.bfloat16
AF = mybir.ActivationFunctionType
ALU = mybir.AluOpType
AX = mybir.AxisListType

D = 1024
DEPTH = 2
NCORE = 8
PAST_LEN = 16384
ALPHA = (2 * DEPTH) ** 0.25
EPS = 1e-5
N_IN = 3096
NEG = -1.0e5
C_ID, C_TRI, C_NEG, C_DM, C_GQ, C_GK, C_AL, C_HM4, C_HM2, C_SEGALL, C_SEGIND, C_END = (
    0, 128, 256, 384, 896, 1152, 1156, 1158, 1162, 1164, 1292, 1308)


class Buf:
    __slots__ = ("name", "last_w", "readers")

    def __init__(self, name):
        self.name = name
        self.last_w = None
        self.readers = {}


class T:
    def __init__(self, h, name):
        self.h = h
        self.b = Buf(name)

    def __getitem__(self, k):
        return self.h[k]


class View:
    def __init__(self, parent, ap):
        self.h = ap
        self.b = parent.b

    def __getitem__(self, k):
        return self.h[k]


def _b(x):
    return x.b if isinstance(x, (T, View)) else x


class Sched:
    ENG = ("pe", "act", "dve", "pool", "sp")

    def __init__(self, nc, stack, n_dma_sems=8):
        self.nc = nc
        self.lists = {e: [] for e in self.ENG}
        self.sem = {e: stack.enter_context(nc.semaphore("s_" + e)) for e in ("pe", "act", "dve", "pool")}
        self.cnt = {e: 0 for e in ("pe", "act", "dve", "pool")}
        self.dq = {}
        for q in ("sp", "pool"):
            sems = [stack.enter_context(nc.semaphore("d_%s%d" % (q, i))) for i in range(n_dma_sems)]
            self.dq[q] = {"sems": sems, "cnt": [0] * n_dma_sems, "next": 0}
        self.waited = {e: {} for e in self.ENG}
        self.n = 0

    def _deps(self, reads, writes):
        deps = []
        for b in reads:
            if b.last_w is not None:
                deps.append(b.last_w)
        for b in writes:
            if b.last_w is not None:
                deps.append(b.last_w)
            deps.extend(b.readers.values())
        return deps

    def _waits(self, e, deps):
        waits = []
        for (key, sem, val, eng) in deps:
            if eng == "pe" and e == "pe":
                continue
            if self.waited[e].get(key, 0) >= val:
                continue
            self.waited[e][key] = val
            waits.append((sem, val))
        return waits

    def op(self, e, fn, r=(), w=()):
        reads = [_b(x) for x in r]
        writes = [_b(x) for x in w]
        waits = self._waits(e, self._deps(reads, writes))
        self.cnt[e] += 1
        self.n += 1
        self.lists[e].append((waits, fn, self.sem[e], 1))
        tok = (e, self.sem[e], self.cnt[e], e)
        for b in reads:
            b.readers[e] = tok
        for b in writes:
            b.last_w = tok
            b.readers = {}

    def dma(self, q, fn, r=(), w=()):
        reads = [_b(x) for x in r]
        writes = [_b(x) for x in w]
        d = self.dq[q]
        i = d["next"]
        d["next"] = (i + 1) % len(d["sems"])
        sem = d["sems"][i]
        deps = self._deps(reads, writes)
        key = "d_%s%d" % (q, i)
        if d["cnt"][i] > 0:
            deps.append((key, sem, d["cnt"][i] * 16, "dma"))
        waits = self._waits(q, deps)
        d["cnt"][i] += 1
        self.n += 1
        self.lists[q].append((waits, fn, sem, 16))
        tok = (key, sem, d["cnt"][i] * 16, "dma")
        for b in reads:
            b.readers[key] = tok
        for b in writes:
            b.last_w = tok
            b.readers = {}

    def final_wait(self, q, bufs):
        deps = []
        for b in bufs:
            b = _b(b)
            if b.last_w is not None:
                deps.append(b.last_w)
        self.lists[q].append((self._waits(q, deps), None, None, 0))

    def emit(self, block):
        def run(eng, lst):
            for waits, fn, sem, inc in lst:
                for (s, v) in waits:
                    eng.wait_ge(s, v)
                if fn is not None:
                    fn(eng).then_inc(sem, inc)

        @block.tensor
        def _(eng):
            run(eng, self.lists["pe"])

        @block.scalar
        def _(eng):
            run(eng, self.lists["act"])

        @block.vector
        def _(eng):
            run(eng, self.lists["dve"])

        @block.gpsimd
        def _(eng):
            run(eng, self.lists["pool"])

        @block.sync
        def _(eng):
            run(eng, self.lists["sp"])


def make_consts(kind):
    p = np.arange(128)
    if kind == "p":
        seg = np.zeros(128, np.int64)
        pos = p.copy()
        L = 128
        nseg = 1
    else:
        seg = p // 8
        pos = p % 8
        L = 8
        nseg = 16
    same = seg[:, None] == seg[None, :]
    c = np.zeros((128, C_END), np.float32)
    c[:, C_ID:C_ID + 128] = np.eye(128)
    caus = (p[:, None] <= p[None, :]) & same
    c[:, C_TRI:C_TRI + 128] = caus
    c[:, C_NEG:C_NEG + 128] = np.where(caus, 0.0, NEG)
    gam = 1.0 - 2.0 ** (-5.0 - np.arange(4, dtype=np.float64))
    lg = np.log(gam)
    for h in range(4):
        dm = np.where(caus, np.exp(lg[h] * (pos[None, :] - pos[:, None]).astype(np.float64)), 0.0)
        c[:, C_DM + h * 128:C_DM + (h + 1) * 128] = dm
        c[:, C_GK + h] = np.exp(lg[h] * (L - 1 - pos))
    for u in range(2):
        for hl in range(2):
            h = 2 * u + hl
            c[hl * 64:(hl + 1) * 64, C_GQ + u * 128:C_GQ + (u + 1) * 128] = np.exp(lg[h] * (pos[None, :] + 1.0))
            c[hl * 64:(hl + 1) * 64, C_AL + u] = np.exp(lg[h] * L)
    for h in range(4):
        c[:, C_HM4 + h] = (p // 32 == h)
    for h in range(2):
        c[:, C_HM2 + h] = (p // 64 == h)
    c[:, C_SEGALL:C_SEGALL + 128] = same
    for s in range(nseg):
        c[:, C_SEGIND + s] = (seg == s)
    return c


def make_trig():
    half = 32
    inv = (np.float32(10000.0) ** (-np.arange(half, dtype=np.float32) / np.float32(half))).astype(np.float32)
    out = np.zeros((17, 128, 128), np.float32)
    for t in range(17):
        if t < 16:
            pos = (t * 128 + np.arange(128)).astype(np.float32)
        else:
            pos = (PAST_LEN + (np.arange(128) % 8)).astype(np.float32)
        ang = (pos[:, None] * inv[None, :]).astype(np.float32)
        co = np.cos(ang).astype(np.float32)
        si = np.sin(ang).astype(np.float32)
        out[t, :, 0:32] = co
        out[t, :, 32:64] = co
        out[t, :, 64:96] = -si
        out[t, :, 96:128] = si
    return out


def build(n_layers=DEPTH, groups=None, stop_after=None):
    nc = bass.Bass("TRN2", target_bir_lowering=False)
    if groups is None:
        groups = [("p", [0, 1, 2, 3]), ("p", [4, 5, 6, 7]), ("p", [8, 9, 10, 11]), ("p", [12, 13, 14, 15]), ("s", [16])]
    GM = max(len(g[1]) for g in groups)
    NTM = GM * 128

    def din(name, shape, dt=F32):
        return nc.dram_tensor(name, list(shape), dt, kind="ExternalInput").ap()

    def dout(name, shape, dt=F32):
        return nc.dram_tensor(name, list(shape), dt, kind="ExternalOutput").ap()

    x_all = din("x_all", [17, 128, D])
    cT = din("cT", [128, 8, 18])
    consts_p = din("consts_p", [128, C_END])
    consts_s = din("consts_s", [128, C_END])
    trig = din("trig", [17, 128, 128])
    st_gla = din("st_gla", [2, 16, 4, 32, 64])
    st_ret = din("st_ret", [2, 16, 4, 64, 64])
    st_ssd = din("st_ssd", [2, 16, 8, 64, 64])
    st_conv = din("st_conv", [2, 128, 6, 16, 3])
    w_ada = din("w_ada", [2, D, 6 * D])
    b_adaT = din("b_adaT", [128, 2, 48])
    w_in = din("w_in", [2, D, N_IN])
    w_gate = din("w_gate", [16, 2, 128])
    b_gateT = din("b_gateT", [128, 2])
    rows = din("rows", [2, 1, 1048])
    conv_wT = din("conv_wT", [128, 2, 6, 4])
    conv_bT = din("conv_bT", [128, 2, 6])
    w_out = din("w_out", [2, D, D])
    ln_rows = din("ln_rows", [2, 4, 1, D])
    w_rt = din("w_rt", [128, 2, 8, 20])
    b_rt = din("b_rt", [2, 1, 20])
    moe_w1 = din("moe_w1", [2, 16, D, 256])
    moe_w3 = din("moe_w3", [2, 16, D, 256])
    moe_w2 = din("moe_w2", [2, 16, 256, D])

    y_all = dout("y_all", [17, 128, D])
    o_gla_p = dout("o_gla_p", [2, 128, 64])
    o_ret_p = dout("o_ret_p", [2, 2, 128, 64])
    o_ssd_p = dout("o_ssd_p", [2, 4, 128, 64])
    o_conv_p = dout("o_conv_p", [2, 3, 768])
    o_gla_s = dout("o_gla_s", [2, 128, 16, 64])
    o_ret_s = dout("o_ret_s", [2, 2, 128, 16, 64])
    o_ssd_s = dout("o_ssd_s", [2, 4, 128, 16, 64])
    o_conv_s = dout("o_conv_s", [2, 48, 768])
    outs = []
    if os.environ.get("KDBG") in ("1", "2"):
        dbg_out = dout("dbg_out", [128, 8, 128])

    with ExitStack() as st:
        S = Sched(nc, st)
        _cnt = [0]

        def sb(shape, dt=F32, name=None):
            _cnt[0] += 1
            nm = "%s_%d" % (name or "t", _cnt[0])
            return T(st.enter_context(nc.sbuf_tensor(nm, list(shape), dt)), nm)

        def MM(out, lhsT, rhs, start, stop, r, w):
            S.op("pe", lambda e: e.matmul(out, lhsT, rhs, start=start, stop=stop), r, w)

        def TR(out, in_, ident, r, w):
            S.op("pe", lambda e: e.transpose(out, in_, ident), r, w)

        def ACT(out, in_, func, r, w, bias=None, scale=None):
            kw = {}
            if bias is not None:
                kw["bias"] = bias
            if scale is not None:
                kw["scale"] = scale
            S.op("act", lambda e: e.activation(out=out, in_=in_, func=func, **kw), r, w)

        def TTo(eng, out, a, b, op, r, w):
            S.op(eng, lambda e: e.tensor_tensor(out=out, in0=a, in1=b, op=op), r, w)

        def TSo(eng, out, a, s1, op0, r, w, s2=None, op1=None):
            if op1 is None:
                S.op(eng, lambda e: e.tensor_scalar(out, a, s1, None, op0), r, w)
            else:
                S.op(eng, lambda e: e.tensor_scalar(out, a, s1, s2, op0, op1), r, w)

        def STT(out, a, scalar, b, op0, op1, r, w):
            S.op("dve", lambda e: e.scalar_tensor_tensor(out=out, in0=a, scalar=scalar, in1=b, op0=op0, op1=op1), r, w)

        def CP(eng, out, in_, r, w):
            S.op(eng, lambda e: e.tensor_copy(out=out, in_=in_), r, w)

        def MSET(eng, ap, val, w):
            S.op(eng, lambda e: e.memset(ap, val), (), w)

        def DMA(q, out, in_, r, w):
            S.dma(q, lambda e: e.dma_start(out=out, in_=in_), r, w)

        banks = []
        for i in range(8):
            banks.append(T(st.enter_context(nc.psum_tensor("bank%d" % i, [128, 512], F32)), "bank%d" % i))
        _bk = [0]

        _free = []
        _mode = [False]

        def PSA():
            assert _free, "out of PSUM banks in chain phase"
            return _free.pop(0)

        def PSF(b):
            _free.append(b)

        def run_chains(gens, interleave):
            _free[:] = [banks[(_bk[0] + i) % 8] for i in range(8)]
            if not interleave:
                for gch in gens:
                    for _ in gch:
                        pass
            else:
                active = list(gens)
                wts = {id(gch): w for gch, w in zip(gens, (1.5, 1.0, 1.0, 0.3))}
                cred = {id(gch): 0.0 for gch in gens}
                while active:
                    for gch in list(active):
                        cred[id(gch)] += wts[id(gch)]
                        while cred[id(gch)] >= 1.0 and gch in active:
                            cred[id(gch)] -= 1.0
                            try:
                                next(gch)
                            except StopIteration:
                                active.remove(gch)
            assert len(_free) == 8, len(_free)

        def PS():
            b = banks[_bk[0] % 8]
            _bk[0] += 1
            return b

        cst = sb([128, C_END], name="cst")
        identb = sb([128, 128], BF16, name="identb")
        trig_t = sb([128, GM, 128], name="trig")
        cT_t = sb([128, 8, 18], name="cT")
        scT = sb([128, 8, 18], BF16, name="scT")
        mod = [sb([128, 48, 18], name="mod%d" % l) for l in range(2)]
        badaT = sb([128, 2, 48], name="badaT")
        wg_t = sb([16, 2, 128], name="wg")
        nbg_t = sb([128, 2], name="nbg")
        rows_1 = sb([128, 1048], name="rows")
        rows_t = [rows_1, rows_1]
        a_neg_1 = sb([128, 8], name="aneg")
        a_neg = [a_neg_1, a_neg_1]
        cw_t = sb([128, 2, 6, 4], name="cw")
        cb_t = sb([128, 2, 6], name="cb")
        wrt_t = sb([128, 2, 8, 20], name="wrt")
        brt_t = [sb([128, 20], name="brt%d" % l) for l in range(2)]
        Pb = [sb([128, 512], name="P%d" % i) for i in range(4)]
        Qb = [sb([128, 1024], name="Q%d" % i) for i in range(4)]
        lnr = [Qb[0], Qb[1]]
        snew = sb([128, 16, 64], name="snew")
        Sg = [sb([128, 64], name="Sg%d" % l) for l in range(2)]
        Sr = [[sb([128, 64], name="Sr%d_%d" % (l, u)) for u in range(2)] for l in range(2)]
        Ss = [[sb([128, 64], name="Ss%d_%d" % (l, u)) for u in range(4)] for l in range(2)]
        chist = [sb([128, 6, 3], name="chist%d" % l) for l in range(2)]
        X = [sb([128, D], name="X%d" % g) for g in range(GM)]
        S0 = View(X[1], X[1][:].rearrange("p (s v) -> p s v", s=16)) if GM > 1 else sb([128, 16, 64], name="S0")
        HT = sb([128, 8, NTM], BF16, name="HT")
        HTb = [Buf("HT%d" % g) for g in range(GM)]
        FM = sb([128, 8, NTM], name="FMacc")
        NSLOT = 5
        slots = [sb([128, 8, 512], BF16, name="slot%d" % i) for i in range(NSLOT)]
        _sl = [0]

        plan = []
        _wi = [0, 0]
        PF = 3

        def kcv(ap2d):
            return ap2d.rearrange("(kc p) n -> p kc n", p=128)

        def plan_ada(l):
            return [[(slice(0, 512), kcv(w_ada[l])[:, :, j * 512:(j + 1) * 512])] for j in range(12)]

        def plan_layer(l, with_ada=None):
            p = []
            for (c0, c1) in ((0, 512), (512, 784), (784, 1296), (1296, 1808), (1808, 2320), (2320, 2832), (2832, 3096)):
                p.append([(slice(0, c1 - c0), kcv(w_in[l])[:, :, c0:c1])])
            if with_ada is not None:
                p.extend(plan_ada(with_ada))
            for hf in range(2):
                p.append([(slice(0, 512), kcv(w_out[l])[:, :, hf * 512:(hf + 1) * 512])])
            return p

        def plan_moe(l):
            p = []
            for ex in range(16):
                p.append([(slice(0, 256), kcv(moe_w1[l, ex])), (slice(256, 512), kcv(moe_w3[l, ex]))])
                p.append([("w2", moe_w2[l, ex].rearrange("(fc p) n -> p fc n", p=128))])
            return p

        plan.extend(plan_ada(0))
        ADA_IN_CHAIN = (n_layers == 2 and groups[0][0] == "p")
        if not ADA_IN_CHAIN:
            for l in range(1, n_layers):
                plan.extend(plan_ada(l))
        for gi_, (kind_, tiles_) in enumerate(groups):
            for l in range(n_layers):
                plan.extend(plan_layer(l, with_ada=(1 if (ADA_IN_CHAIN and gi_ == 0 and l == 0) else None)))
                if stop_after != ("mixer", l):
                    plan.extend(plan_moe(l))
                if stop_after is not None and stop_after[1] == l:
                    break

        def w2view(sl):
            return sl[:].rearrange("p a b -> p (a b)")[:, 0:2048].rearrange("p (a b) -> p a b", a=2)

        def next_slot():
            while _wi[1] < len(plan) and _wi[1] <= _wi[0] + PF:
                i = _wi[1]
                sl = slots[i % NSLOT]
                for (cs, src) in plan[i]:
                    if isinstance(cs, str):
                        DMA("pool", w2view(sl), src, [], [sl])
                    else:
                        DMA("pool", sl[:, :, cs], src, [], [sl])
                _wi[1] += 1
            sl = slots[_wi[0] % NSLOT]
            _wi[0] += 1
            return sl

        ID = cst[:, C_ID:C_ID + 128]

        DMA("sp", cst[:], consts_p, [], [cst])
        DMA("sp", cT_t[:], cT, [], [cT_t])
        DMA("sp", badaT[:], b_adaT, [], [badaT])
        DMA("sp", wg_t[:], w_gate, [], [wg_t])
        DMA("sp", nbg_t[:], b_gateT, [], [nbg_t])
        DMA("sp", cw_t[:], conv_wT, [], [cw_t])
        DMA("sp", cb_t[:], conv_bT, [], [cb_t])
        DMA("sp", wrt_t[:], w_rt, [], [wrt_t])
        for l in range(2):
            DMA("sp", brt_t[l][:], b_rt[l].partition_broadcast(128), [], [brt_t[l]])
        CP("dve", identb[:], ID, [cst], [identb])
        TSo("dve", nbg_t[:], nbg_t[:], -1.0, ALU.mult, [nbg_t], [nbg_t])
        for l in range(2):
            for t_ in [Sg[l]] + Sr[l] + Ss[l] + [chist[l]]:
                MSET("dve", t_[:], 0.0, [t_])

        ACT(scT[:], cT_t[:], AF.Silu, [cT_t], [scT])

        def emit_ada(l, alloc, free):
            for j in range(12):
                sl = next_slot()
                pb = alloc()
                for mc in range(4):
                    for kc in range(8):
                        MM(pb[:, mc * 18:(mc + 1) * 18], sl[:, kc, mc * 128:(mc + 1) * 128], scT[:, kc, :],
                           kc == 0, kc == 7, [sl, scT], [pb])
                TTo("dve", mod[l][:, j * 4:(j + 1) * 4, :], pb[:, 0:72].rearrange("p (a b) -> p a b", a=4),
                    badaT[:, l, j * 4:(j + 1) * 4].unsqueeze(2).to_broadcast([128, 4, 18]), ALU.add,
                    [pb, badaT], [mod[l]])
                free(pb)
                yield
            for comp in (1, 4):
                TSo("dve", mod[l][:, comp * 8:(comp + 1) * 8, :], mod[l][:, comp * 8:(comp + 1) * 8, :], 1.0, ALU.add,
                    [mod[l]], [mod[l]])

        for _ in emit_ada(0, PS, lambda b: None):
            pass
        if not ADA_IN_CHAIN:
            for l_ in range(1, n_layers):
                for _ in emit_ada(l_, PS, lambda b: None):
                    pass

        def w_in_chunk(l, c0, c1):
            return next_slot()

        def mod_evac(kind, l, sc, sh, kc, out_ap, ps_ap, r, w, tmp):
            if kind == "p":
                ACT(out_ap, ps_ap, AF.Identity, r + [mod[l]], w, bias=mod[l][:, sh * 8 + kc, 0:1], scale=mod[l][:, sc * 8 + kc, 0:1])
            else:
                TTo("dve", tmp[:, 0:128].rearrange("p (s t) -> p s t", s=16), ps_ap.rearrange("p (s t) -> p s t", s=16),
                    mod[l][:, sc * 8 + kc, 1:17].unsqueeze(2).to_broadcast([128, 16, 8]), ALU.mult, r + [mod[l]], [tmp])
                TTo("dve", out_ap.rearrange("p (s t) -> p s t", s=16), tmp[:, 0:128].rearrange("p (s t) -> p s t", s=16),
                    mod[l][:, sh * 8 + kc, 1:17].unsqueeze(2).to_broadcast([128, 16, 8]), ALU.add, [tmp, mod[l]], w)

        def gate_evac(kind, l, gc, kc, out_ap, in_ap, ntok, r, w):
            if kind == "p":
                ACT(out_ap, in_ap, AF.Copy, r + [mod[l]], w, scale=mod[l][:, gc * 8 + kc, 0:1])
            else:
                TTo("dve", out_ap.rearrange("p (s t) -> p s t", s=16), in_ap.rearrange("p (s t) -> p s t", s=16),
                    mod[l][:, gc * 8 + kc, 1:17].unsqueeze(2).to_broadcast([128, 16, 8]), ALU.mult, r + [mod[l]], w)

        tmpA = sb([128, 512], name="tmpA")
        tmpB = sb([128, 512], name="tmpB")
        tmpC = sb([128, 512], name="tmpC")
        sm = sb([128, 64], name="small")
        stats = sb([128, 2, 6], name="bnst")

        def make_HT(kind, l, G, sc, sh, h32=None):
            for g in range(G):
                for half in range(2):
                    pb = PS()
                    for q in range(4):
                        kc = half * 4 + q
                        TR(pb[:, q * 128:(q + 1) * 128], X[g][:, kc * 128:(kc + 1) * 128], ID, [X[g], cst], [pb])
                    for q in range(4):
                        kc = half * 4 + q
                        if h32 is None:
                            mod_evac(kind, l, sc, sh, kc, HT[:, kc, g * 128:(g + 1) * 128], pb[:, q * 128:(q + 1) * 128],
                                     [pb], [HTb[g]], tmpA)
                        else:
                            mod_evac(kind, l, sc, sh, kc, h32[g][:, kc, :], pb[:, q * 128:(q + 1) * 128],
                                     [pb], [h32[g]], tmpA)
                            if kind == "p":
                                mod_evac(kind, l, sc, sh, kc, HT[:, kc, g * 128:(g + 1) * 128], pb[:, q * 128:(q + 1) * 128],
                                         [pb], [HTb[g]], tmpA)
                            else:
                                CP("dve", HT[:, kc, g * 128:(g + 1) * 128], h32[g][:, kc, :], [h32[g]], [HTb[g]])

        def fm_proj(sl, col0, ncol, NT, G, evac):
            pb = PS()
            for kc in range(8):
                MM(pb[0:ncol, 0:NT], sl[:, kc, col0:col0 + ncol], HT[:, kc, 0:NT], kc == 0, kc == 7,
                   [sl] + HTb[:G], [pb])
            evac(pb)

        def tm_proj(sl, col0, ncol, g, evac):
            pb = PS()
            for kc in range(8):
                MM(pb[:, 0:ncol], HT[:, kc, g * 128:(g + 1) * 128], sl[:, kc, col0:col0 + ncol], kc == 0, kc == 7,
                   [sl, HTb[g]], [pb])
            evac(pb)

        def layer_norm_tile(g, pbs, lg, lb):
            u = tmpU
            for hf in range(2):
                STT(u[:, hf * 512:(hf + 1) * 512], X[g][:, hf * 512:(hf + 1) * 512], float(ALPHA), pbs[hf][:, 0:512],
                    ALU.mult, ALU.add, [X[g], pbs[hf]], [u])
            for hf in range(2):
                S.op("dve", lambda e, hf=hf: e.bn_stats(stats[:, hf, :], u[:, hf * 512:(hf + 1) * 512]), [u], [stats])
            S.op("dve", lambda e: e.bn_aggr(sm[:, 0:2], stats[:].rearrange("p a b -> p (a b)")), [stats], [sm])
            TSo("dve", sm[:, 2:3], sm[:, 1:2], float(EPS), ALU.add, [sm], [sm])
            ACT(sm[:, 3:4], sm[:, 2:3], AF.Ln, [sm], [sm])
            ACT(sm[:, 4:5], sm[:, 3:4], AF.Exp, [sm], [sm], scale=-0.5)
            TSo("dve", u[:], u[:], sm[:, 0:1], ALU.subtract, [u, sm], [u], s2=sm[:, 4:5], op1=ALU.mult)
            TTo("dve", u[:], u[:], lg[:], ALU.mult, [u, lg], [u])
            TTo("dve", X[g][:], u[:], lb[:], ALU.add, [u, lb], [X[g]])

        tmpU = Qb[3]

        Km = sb([128, 512], BF16, name="Km")
        Kms = [sb([128, 512], BF16, name="Kms%d" % i) for i in range(2)]
        Qms = sb([128, 16, 128], BF16, name="Qms")
        scm = sb([128, 512], BF16, name="scm")
        Sbf = sb([128, 16, 64], BF16, name="Sbf")

        cur_kind = ["p"]
        arena = {}

        def A(name, shape, dt=F32):
            if name not in arena:
                arena[name] = sb(shape, dt, name=name)
            return arena[name]

        arena["qT"] = Pb[0]
        arena["kT"] = Pb[1]
        arena["laT"] = Pb[2]
        arena["gaT"] = Pb[3]
        FMf = FM[:].rearrange("p a b -> p (a b)")
        arena["cacc"] = View(FM, FMf[:, GM * 896:GM * 1024])
        arena["cacc"].b = Buf("cacc")
        arena["ys"] = View(Qb[2], Qb[2][:, 512:1024])
        arena["ce"] = View(Qb[3], Qb[3][:, 512:1024].rearrange("p (h n) -> p h n", h=8))
        arena["sact"] = Pb[0]
        arena["tact"] = Pb[1]
        for g_ in range(GM):
            arena["rq%d" % g_] = View(FM, FMf[:, g_ * 256:(g_ + 1) * 256])
            arena["rk%d" % g_] = View(FM, FMf[:, GM * 256 + g_ * 256:GM * 256 + (g_ + 1) * 256])
            arena["srg%d" % g_] = View(FM, FMf[:, GM * 512 + g_ * 256:GM * 512 + (g_ + 1) * 256])
            arena["vr%d" % g_] = View(FM, FMf[:, GM * 768 + g_ * 128:GM * 768 + (g_ + 1) * 128].bitcast(BF16))
            arena["szs%d" % g_] = View(Qb[g_], Qb[g_][:, 0:512])
            arena["h32_%d" % g_] = View(Qb[g_], Qb[g_][:].rearrange("p (a b) -> p a b", a=8))
        arena["snew"] = snew
        arena["sns"] = snew
        arena["dtr"] = View(snew, snew[:].rearrange("p a b -> p (a b)").rearrange("p (h j) -> p h j", h=8))
        arena["rqtm"] = A("qtm", [128, 4, 128], BF16)
        arena["sel"] = View(Qms, Qms[0:16, :, :])
        arena["c32"] = View(tmpA, tmpA[0:16, 0:128])
        arena["me"] = View(tmpB, tmpB[:, 0:64])
        arena["rt"] = View(tmpB, tmpB[:, 64:128])
        arena["lg"] = View(tmpB, tmpB[:, 128:148])
        cvoA = View(Qb[0], Qb[0][:, 512:1024])
        cvoB = View(Qb[1], Qb[1][:, 512:768])

        for gi, (kind, tiles) in enumerate(groups):
            G = len(tiles)
            NT = G * 128
            nseg = 1 if kind == "p" else 16
            SL = 128 // nseg
            if kind != cur_kind[0]:
                DMA("sp", cst[:], consts_s, [], [cst])
                cur_kind[0] = kind
            for g in range(G):
                DMA("sp", X[g][:], x_all[tiles[g]], [], [X[g]])
                DMA("sp", trig_t[:, g, :], trig[tiles[g]], [], [trig_t])
            last_prompt = (kind == "p" and tiles[-1] == 15)

            KST = int(os.environ.get("KSTAGE", "9"))
            for l in range(n_layers):
                if KST == 0:
                    continue
                if kind == "s" and os.environ.get("KSG") == "0":
                    continue
                TRI = cst[:, C_TRI:C_TRI + 128]
                DMA("sp", rows_t[l][:], rows[l].partition_broadcast(128), [], [rows_t[l]])
                ACT(a_neg[l][:], rows_t[l][:, 1032:1040], AF.Exp, [rows_t[l]], [a_neg[l]])
                TSo("dve", a_neg[l][:], a_neg[l][:], -1.0, ALU.mult, [a_neg[l]], [a_neg[l]])
                make_HT(kind, l, G, 1, 0)
                KSG = int(os.environ.get("KSG", "9")) if kind == "s" else 9
                if KSG <= 1:
                    continue
                mergedT = A("mergedT", [128, 8, NTM], BF16)
                mTb = [Buf("mT%d" % g) for g in range(GM)]

                QD = bass.AP(Qms[:].tensor, 0, [[Qms[:].ap[0][0], 128], [136, 16], [1, 8]])

                def U_mm(pu, items):
                    n = len(items)
                    for i, (lk, lkT, va, vT) in enumerate(items):
                        if kind == "p":
                            MM(pu[0][:, 0:64], lk, va, i == 0, i == n - 1, [lkT, vT], [pu[0]])
                        else:
                            for hf in range(2):
                                TTo("dve", Kms[hf][:].rearrange("p (s v) -> p s v", s=8), va.unsqueeze(1).to_broadcast([128, 8, 64]),
                                    cst[:, C_SEGIND + hf * 8:C_SEGIND + hf * 8 + 8].unsqueeze(2).to_broadcast([128, 8, 64]), ALU.mult,
                                    [vT, cst], [Kms[hf]])
                                MM(pu[hf][:, 0:512], lk, Kms[hf][:], i == 0, i == n - 1, [lkT, Kms[hf]], [pu[hf]])

                def emit_merged(g, mo, c0, nchunk, r):
                    pb = PSA()
                    for q in range(nchunk):
                        TR(pb[:, q * 128:(q + 1) * 128], mo[:, q * 128:(q + 1) * 128], ID, r + [cst], [pb])
                    ACT(mergedT[:, c0:c0 + nchunk, g * 128:(g + 1) * 128],
                        pb[:, 0:nchunk * 128].rearrange("p (a b) -> p a b", a=nchunk), AF.Copy, [pb], [mTb[g]])
                    PSF(pb)

                qT = A("qT", [128, NTM])
                kT = A("kT", [128, NTM])
                gaT = A("gaT", [16, NTM])
                vg = [A("vg%d" % g, [128, 256], BF16) for g in range(GM)]
                sgr = [A("sgr%d" % g, [128, 256]) for g in range(GM)]
                laT = A("laT", [128, NTM])
                sl = w_in_chunk(l, 0, 512)
                fm_proj(sl, 0, 128, NT, G, lambda pb: ACT(qT[:, 0:NT], pb[:, 0:NT], AF.Copy, [pb], [qT], scale=float(32 ** -0.5)))
                fm_proj(sl, 128, 128, NT, G, lambda pb: CP("dve", kT[:, 0:NT], pb[:, 0:NT], [pb], [kT]))
                for g in range(G):
                    tm_proj(sl, 256, 256, g, lambda pb, g=g: ACT(vg[g][:], pb[:, 0:256], AF.Copy, [pb], [vg[g]]))
                sl = w_in_chunk(l, 512, 784)
                fm_proj(sl, 0, 16, NT, G, lambda pb: CP("dve", gaT[0:16, 0:NT], pb[0:16, 0:NT], [pb], [gaT]))
                for g in range(G):
                    tm_proj(sl, 16, 256, g, lambda pb, g=g: ACT(sgr[g][:], pb[:, 0:256], AF.Silu, [pb], [sgr[g]]))
                pb = PS()
                MM(pb[:, 0:NT], wg_t[:, l, :], gaT[0:16, 0:NT], True, True, [wg_t, gaT], [pb])
                ACT(laT[:, 0:NT], pb[:, 0:NT], AF.Exp, [pb, nbg_t], [laT], bias=nbg_t[:, l:l + 1], scale=-1.0)
                TSo("dve", laT[:, 0:NT], laT[:, 0:NT], 1.0, ALU.add, [laT], [laT])
                ACT(laT[:, 0:NT], laT[:, 0:NT], AF.Ln, [laT], [laT])
                def gla_chain():
                    for g in range(G):
                        tk = slice(g * 128, (g + 1) * 128)
                        la_tm = A("la_tm", [128, 128])
                        eg = A("eg", [128, 128])
                        eng_ = A("eng", [128, 128])
                        alast = A("alast", [128, 16])
                        qtm = A("qtm", [128, 4, 128], BF16)
                        ktl = A("ktl", [128, 128], BF16)
                        pb = PSA()
                        TR(pb[:, 0:128], laT[:, tk], ID, [laT, cst], [pb])
                        ACT(la_tm[:], pb[:, 0:128], AF.Copy, [pb], [la_tm], scale=-1.0 / 16.0)
                        PSF(pb)
                        yield
                        pg = PSA()
                        MM(pg[:, 0:128], la_tm[:], TRI, True, True, [la_tm, cst], [pg])
                        ACT(eg[:], pg[:, 0:128], AF.Exp, [pg], [eg])
                        ACT(eng_[:], pg[:, 0:128], AF.Exp, [pg], [eng_], scale=-1.0)
                        ACT(alast[:, 0:nseg], pg[:, SL - 1:128:SL], AF.Exp, [pg], [alast])
                        PSF(pg)
                        yield
                        for h in range(4):
                            STT(qtm[:, h, :], qT[:, tk], cst[:, C_HM4 + h:C_HM4 + h + 1], eg[:], ALU.mult, ALU.mult,
                                [qT, cst, eg], [qtm])
                        TTo("pool", ktl[:], kT[:, tk], eng_[:], ALU.mult, [kT, eng_], [ktl])
                        yield
                        pk = PSA()
                        pkb = pk[:].bitcast(BF16)
                        TR(pkb[:, 0:128], ktl[:], identb[:], [ktl, identb], [pk])
                        if g == 0 and l == 0 and gi == 0:
                            pass
                        MSET("pool", Km[:], 0.0, [Km])
                        for h in range(4):
                            ACT(Km[:, h * 128 + h * 32:h * 128 + h * 32 + 32], pkb[:, h * 32:(h + 1) * 32], AF.Copy, [pk], [Km])
                        PSF(pk)
                        yield
                        psc = PSA()
                        for h in range(4):
                            MM(psc[:, h * 128:(h + 1) * 128], ktl[:], qtm[:, h, :], True, True, [ktl, qtm], [psc])
                        TTo("dve", scm[:].rearrange("p (h i) -> p h i", h=4), psc[:].rearrange("p (h i) -> p h i", h=4),
                            TRI.unsqueeze(1).to_broadcast([128, 4, 128]), ALU.mult, [psc, cst], [scm])
                        PSF(psc)
                        yield
                        po = PSA()
                        if kind == "p":
                            CP("pool", Sbf[:, 0, :], Sg[l][:], [Sg[l]], [Sbf])
                        else:
                            DMA("sp", S0[:], st_gla[l].rearrange("s h k v -> (h k) s v"), [], [S0])
                            CP("dve", Sbf[:], S0[:], [S0], [Sbf])
                        for h in range(4):
                            MM(po[:, h * 64:(h + 1) * 64], scm[:, h * 128:(h + 1) * 128], vg[g][:, h * 64:(h + 1) * 64], True, False,
                               [scm, vg[g]], [po])
                            if kind == "p":
                                MM(po[:, h * 64:(h + 1) * 64], qtm[:, h, :], Sbf[:, 0, :], False, True, [qtm, Sbf], [po])
                            else:
                                CP("dve", QD, qtm[:, h, :].rearrange("p (s t) -> p s t", s=16), [qtm], [Qms])
                                for s in range(16):
                                    MM(po[:, h * 64:(h + 1) * 64], Qms[:, s, :], Sbf[:, s, :], False, s == 15, [Qms, Sbf], [po])
                        yield
                        og = A("og", [128, 256])
                        ACT(og[:], po[:, 0:256], AF.Copy, [po], [og])
                        PSF(po)
                        pu = [PSA(), PSA() if nseg > 8 else None]
                        U_mm(pu, [(Km[:, h * 128:(h + 1) * 128], Km, vg[g][:, h * 64:(h + 1) * 64], vg[g]) for h in range(4)])
                        if kind == "p":
                            yield
                            TTo("dve", tmpB[:, 256:320], pu[0][:, 0:64], Sg[l][:], ALU.add, [pu[0], Sg[l]], [tmpB])
                            TSo("dve", Sg[l][:], tmpB[:, 256:320], alast[:, 0:1], ALU.mult, [tmpB, alast], [Sg[l]])
                            if last_prompt and g == G - 1:
                                DMA("sp", o_gla_p[l], Sg[l][:], [Sg[l]], [])
                                outs.append(Sg[l])
                        else:
                            sn = A("snew", [128, 16, 64])
                            for hf in range(2):
                                TTo("dve", tmpA[:].rearrange("p (s v) -> p s v", s=8), pu[hf][:].rearrange("p (s v) -> p s v", s=8),
                                    S0[:, hf * 8:(hf + 1) * 8, :], ALU.add, [pu[hf], S0], [tmpA])
                                TTo("dve", sn[:, hf * 8:(hf + 1) * 8, :], tmpA[:].rearrange("p (s v) -> p s v", s=8),
                                    alast[:, hf * 8:(hf + 1) * 8].unsqueeze(2).to_broadcast([128, 8, 64]), ALU.mult,
                                    [tmpA, alast], [sn])
                            DMA("sp", o_gla_s[l], sn[:], [sn], [])
                            outs.append(sn)
                        PSF(pu[0])
                        if pu[1] is not None:
                            PSF(pu[1])
                        yield
                        TTo("pool", tmpB[:, 0:256], og[:], og[:], ALU.mult, [og], [tmpB])
                        S.op("dve", lambda e: e.tensor_reduce(sm[:, 8:12], tmpB[:, 0:256].rearrange("p (h v) -> p h v", h=4), AX.X, ALU.add),
                             [tmpB], [sm])
                        TSo("dve", sm[:, 8:12], sm[:, 8:12], 1.0 / 64.0, ALU.mult, [sm], [sm], s2=float(EPS), op1=ALU.add)
                        yield
                        ACT(sm[:, 12:16], sm[:, 8:12], AF.Ln, [sm], [sm])
                        ACT(sm[:, 16:20], sm[:, 12:16], AF.Exp, [sm], [sm], scale=-0.5)
                        TTo("pool", tmpB[:, 0:256], sgr[g][:], rows_t[l][:, 0:256], ALU.mult, [sgr[g], rows_t[l]], [tmpB])
                        TTo("dve", og[:].rearrange("p (h v) -> p h v", h=4), og[:].rearrange("p (h v) -> p h v", h=4),
                            sm[:, 16:20].unsqueeze(2).to_broadcast([128, 4, 64]), ALU.mult, [og, sm], [og])
                        TTo("dve", og[:], og[:], tmpB[:, 0:256], ALU.mult, [og, tmpB], [og])
                        yield
                        emit_merged(g, og, 0, 2, [og])
                        yield


                if KSG <= 2:
                    continue
                rq = [A("rq%d" % g, [128, 256]) for g in range(GM)]
                rk = [A("rk%d" % g, [128, 256]) for g in range(GM)]
                vr = [A("vr%d" % g, [128, 256], BF16) for g in range(GM)]
                srg = [A("srg%d" % g, [128, 256]) for g in range(GM)]
                sl = w_in_chunk(l, 784, 1296)
                for g in range(G):
                    tm_proj(sl, 0, 256, g, lambda pb, g=g: ACT(rq[g][:], pb[:, 0:256], AF.Copy, [pb], [rq[g]]))
                    tm_proj(sl, 256, 256, g, lambda pb, g=g: ACT(rk[g][:], pb[:, 0:256], AF.Copy, [pb], [rk[g]], scale=0.125))
                sl = w_in_chunk(l, 1296, 1808)
                for g in range(G):
                    tm_proj(sl, 0, 256, g, lambda pb, g=g: ACT(vr[g][:], pb[:, 0:256], AF.Copy, [pb], [vr[g]]))
                    tm_proj(sl, 256, 256, g, lambda pb, g=g: ACT(srg[g][:], pb[:, 0:256], AF.Silu, [pb], [srg[g]]))
                def ret_chain():
                    KmR = Kms[0] if kind == "p" else Km
                    scmR = Kms[1] if kind == "p" else scm
                    for g in range(G):
                        cos2 = trig_t[:, g, 0:64].unsqueeze(1).to_broadcast([128, 4, 64])
                        nsin = trig_t[:, g, 64:96].unsqueeze(1).to_broadcast([128, 4, 32])
                        psin = trig_t[:, g, 96:128].unsqueeze(1).to_broadcast([128, 4, 32])
                        ropes = []
                        for nm, src, eng in (("q", rq[g], "dve"), ("k", rk[g], "pool")):
                            t1 = A("rp1" + nm, [128, 256])
                            t2 = A("rp2" + nm, [128, 256])
                            s3 = src[:].rearrange("p (h k) -> p h k", h=4)
                            TTo(eng, t1[:].rearrange("p (h k) -> p h k", h=4), s3, cos2, ALU.mult, [src, trig_t], [t1])
                            TTo(eng, t2[:].rearrange("p (h k) -> p h k", h=4)[:, :, 0:32], s3[:, :, 32:64], nsin, ALU.mult, [src, trig_t], [t2])
                            TTo(eng, t2[:].rearrange("p (h k) -> p h k", h=4)[:, :, 32:64], s3[:, :, 0:32], psin, ALU.mult, [src, trig_t], [t2])
                            TTo(eng, t1[:], t1[:], t2[:], ALU.add, [t1, t2], [t1])
                            ropes.append(t1)
                        qr, kr = ropes
                        yield
                        if kind == "p":
                            qtm = View(Qms, Qms[:, 0:4, :])
                        else:
                            qtm = A("rqtm", [128, 4, 128], BF16)
                        qhm = A("rqhm", [128, 4, 128], BF16)
                        krT = A("krT", [128, 2, 128], BF16)
                        pq = PSA()
                        for u in range(2):
                            TR(pq[:, u * 128:(u + 1) * 128], qr[:, u * 128:(u + 1) * 128], ID, [qr, cst], [pq])
                            TR(pq[:, 256 + u * 128:256 + (u + 1) * 128], kr[:, u * 128:(u + 1) * 128], ID, [kr, cst], [pq])
                        ACT(tmpA[:, 256:512], pq[:, 0:256], AF.Copy, [pq], [tmpA])
                        ACT(krT[:].rearrange("p a b -> p (a b)"), pq[:, 256:512], AF.Copy, [pq], [krT])
                        PSF(pq)
                        yield
                        for u in range(2):
                            for hl in range(2):
                                h = 2 * u + hl
                                TSo("dve", qtm[:, h, :], tmpA[:, 256 + u * 128:256 + (u + 1) * 128], cst[:, C_HM2 + hl:C_HM2 + hl + 1], ALU.mult,
                                    [tmpA, cst], [qtm])
                                STT(qhm[:, h, :], tmpA[:, 256 + u * 128:256 + (u + 1) * 128], cst[:, C_HM2 + hl:C_HM2 + hl + 1],
                                    cst[:, C_GQ + u * 128:C_GQ + (u + 1) * 128], ALU.mult, ALU.mult, [tmpA, cst], [qhm])
                        yield
                        MSET("pool", KmR[:], 0.0, [KmR])
                        for hl in range(2):
                            TTo("dve", KmR[:].rearrange("p (u x) -> p u x", u=2)[:, :, hl * 128 + hl * 64:hl * 128 + hl * 64 + 64],
                                kr[:].rearrange("p (u x) -> p u x", u=2)[:, :, hl * 64:(hl + 1) * 64],
                                cst[:, C_GK + hl:C_GK + hl + 3:2].unsqueeze(2).to_broadcast([128, 2, 64]), ALU.mult,
                                [kr, cst], [KmR])
                        yield
                        psc = PSA()
                        for h in range(4):
                            MM(psc[:, h * 128:(h + 1) * 128], krT[:, h // 2, :], qtm[:, h, :], True, True, [krT, qtm], [psc])
                        TTo("dve", scmR[:], psc[:], cst[:, C_DM:C_DM + 512], ALU.mult, [psc, cst], [scmR])
                        PSF(psc)
                        yield
                        po = PSA()
                        snr = snew
                        for u in range(2):
                            if kind == "p":
                                CP("pool", Sbf[:, 1 + u, :], Sr[l][u][:], [Sr[l][u]], [Sbf])
                            else:
                                DMA("sp", S0[:], st_ret[l][:, 2 * u:2 * u + 2].rearrange("s h k v -> (h k) s v"), [], [S0])
                                CP("dve", Sbf[:], S0[:], [S0], [Sbf])
                            for hl in range(2):
                                h = 2 * u + hl
                                MM(po[:, h * 64:(h + 1) * 64], scmR[:, h * 128:(h + 1) * 128], vr[g][:, h * 64:(h + 1) * 64], True, False,
                                   [scmR, vr[g]], [po])
                                if kind == "p":
                                    MM(po[:, h * 64:(h + 1) * 64], qhm[:, h, :], Sbf[:, 1 + u, :], False, True, [qhm, Sbf], [po])
                                else:
                                    CP("dve", QD, qhm[:, h, :].rearrange("p (s t) -> p s t", s=16), [qhm], [Qms])
                                    for s in range(16):
                                        MM(po[:, h * 64:(h + 1) * 64], Qms[:, s, :], Sbf[:, s, :], False, s == 15, [Qms, Sbf], [po])
                            yield
                            pu = [PSA(), PSA() if nseg > 8 else None]
                            U_mm(pu, [(KmR[:, (2 * u + hl) * 128:(2 * u + hl + 1) * 128], KmR,
                                       vr[g][:, (2 * u + hl) * 64:(2 * u + hl + 1) * 64], vr[g]) for hl in range(2)])
                            if kind == "p":
                                STT(Sr[l][u][:], Sr[l][u][:], cst[:, C_AL + u:C_AL + u + 1], pu[0][:, 0:64], ALU.mult, ALU.add,
                                    [Sr[l][u], cst, pu[0]], [Sr[l][u]])
                                if last_prompt and g == G - 1:
                                    DMA("sp", o_ret_p[l, u], Sr[l][u][:], [Sr[l][u]], [])
                                    outs.append(Sr[l][u])
                            else:
                                for hf in range(2):
                                    STT(snr[:, hf * 8:(hf + 1) * 8, :], S0[:, hf * 8:(hf + 1) * 8, :], cst[:, C_AL + u:C_AL + u + 1],
                                        pu[hf][:].rearrange("p (s v) -> p s v", s=8), ALU.mult, ALU.add, [S0, cst, pu[hf]], [snr])
                                DMA("sp", o_ret_s[l, u], snr[:], [snr], [])
                                outs.append(snr)
                            PSF(pu[0])
                            if pu[1] is not None:
                                PSF(pu[1])
                        yield
                        orr = A("orr", [128, 256])
                        ACT(orr[:], po[:, 0:256], AF.Copy, [po], [orr])
                        PSF(po)
                        yield
                        o3 = orr[:].rearrange("p (h v) -> p h v", h=4)
                        S.op("dve", lambda e, o3=o3: e.tensor_reduce(sm[:, 20:24], o3, AX.X, ALU.add), [orr], [sm])
                        TSo("dve", sm[:, 20:24], sm[:, 20:24], 1.0 / 64.0, ALU.mult, [sm], [sm])
                        TTo("dve", o3, o3, sm[:, 20:24].unsqueeze(2).to_broadcast([128, 4, 64]), ALU.subtract, [orr, sm], [orr])
                        TTo("pool", tmpA[:, 0:256], orr[:], orr[:], ALU.mult, [orr], [tmpA])
                        S.op("dve", lambda e: e.tensor_reduce(sm[:, 24:28], tmpA[:, 0:256].rearrange("p (h v) -> p h v", h=4), AX.X, ALU.add),
                             [tmpA], [sm])
                        yield
                        TSo("dve", sm[:, 24:28], sm[:, 24:28], 1.0 / 64.0, ALU.mult, [sm], [sm], s2=float(EPS), op1=ALU.add)
                        ACT(sm[:, 28:32], sm[:, 24:28], AF.Ln, [sm], [sm])
                        ACT(sm[:, 32:36], sm[:, 28:32], AF.Exp, [sm], [sm], scale=-0.5)
                        TTo("pool", tmpA[:, 0:256], srg[g][:], rows_t[l][:, 256:512], ALU.mult, [srg[g], rows_t[l]], [tmpA])
                        TTo("dve", o3, o3, sm[:, 32:36].unsqueeze(2).to_broadcast([128, 4, 64]), ALU.mult, [orr, sm], [orr])
                        TTo("dve", orr[:], orr[:], tmpA[:, 0:256], ALU.mult, [orr, tmpA], [orr])
                        yield
                        emit_merged(g, orr, 2, 2, [orr])
                        yield


                TT_ = NT // nseg
                szs = [A("szs%d" % g, [128, 512]) for g in range(GM)]
                sx = A("sx", [128, 6, NTM + 48])
                xbc = A("xbcT", [128, 6, NTM], BF16)
                dtx = A("dtx", [128, GM, 8])

                def sxv(c):
                    return sx[:, c, 0:nseg * (TT_ + 3)].rearrange("p (s t) -> p s t", s=nseg)

                if kind == "p":
                    CP("dve", sx[:, :, 0:3], chist[l][:], [chist[l]], [sx])
                else:
                    for c in range(6):
                        DMA("sp", sxv(c)[:, :, 0:3], st_conv[l][:, c], [], [sx])
                sl = w_in_chunk(l, 1808, 2320)
                for g in range(G):
                    tm_proj(sl, 0, 512, g, lambda pb, g=g: ACT(szs[g][:], pb[:, 0:512], AF.Silu, [pb], [szs[g]]))
                need_conv_out = last_prompt or kind == "s"
                for (c0, c1, chs) in ((2320, 2832, [0, 1, 2, 3]), (2832, 3096, [4, 5])):
                    sl = w_in_chunk(l, c0, c1)
                    for qi, c in enumerate(chs):
                        fm_proj(sl, qi * 128, 128, NT, G,
                                lambda pb, c=c: CP("dve", sxv(c)[:, :, 3:3 + TT_], pb[:, 0:NT].rearrange("p (s t) -> p s t", s=nseg),
                                                   [pb], [sx]))
                    if c0 == 2832:
                        for g in range(G):
                            tm_proj(sl, 256, 8, g, lambda pb, g=g: TTo("dve", dtx[:, g, :], pb[:, 0:8], rows_t[l][:, 1024:1032], ALU.add,
                                                                        [pb, rows_t[l]], [dtx]))
                    if need_conv_out:
                        g = G - 1
                        if c0 == 2320:
                            tm_proj(sl, 0, 512, g, lambda pb: ACT(cvoA[:, 0:512], pb[:, 0:512], AF.Copy, [pb], [cvoA]))
                        else:
                            tm_proj(sl, 0, 256, g, lambda pb: ACT(cvoB[:, 0:256], pb[:, 0:256], AF.Copy, [pb], [cvoB]))
                if need_conv_out:
                    if kind == "p":
                        DMA("sp", o_conv_p[l][:, 0:512], cvoA[125:128, :], [cvoA], [])
                        DMA("sp", o_conv_p[l][:, 512:768], cvoB[125:128, :], [cvoB], [])
                    else:
                        for j in range(3):
                            dv = o_conv_s[l].rearrange("(s j) c -> j s c", j=3)[j]
                            DMA("sp", dv[:, 0:512], cvoA[5 + j:128:8, :], [cvoA], [])
                            DMA("sp", dv[:, 512:768], cvoB[5 + j:128:8, :], [cvoB], [])
                if kind == "p":
                    CP("dve", chist[l][:], sx[:, :, NT:NT + 3], [sx], [chist[l]])
                def ssd_chain():
                    dt = A("dt", [128, GM, 8])
                    dta = A("dta", [128, GM, 8])
                    d1 = A("d1", [128, GM, 8])
                    ACT(d1[:, 0:G, :], dtx[:, 0:G, :], AF.Abs, [dtx], [d1])
                    ACT(d1[:, 0:G, :], d1[:, 0:G, :], AF.Exp, [d1], [d1], scale=-1.0)
                    TSo("dve", d1[:, 0:G, :], d1[:, 0:G, :], 1.0, ALU.add, [d1], [d1])
                    ACT(d1[:, 0:G, :], d1[:, 0:G, :], AF.Ln, [d1], [d1])
                    TSo("dve", dt[:, 0:G, :], dtx[:, 0:G, :], 0.0, ALU.max, [dtx], [dt])
                    TTo("dve", dt[:, 0:G, :], dt[:, 0:G, :], d1[:, 0:G, :], ALU.add, [dt, d1], [dt])
                    TTo("dve", dta[:, 0:G, :], dt[:, 0:G, :], a_neg[l][:].unsqueeze(1).to_broadcast([128, G, 8]), ALU.mult,
                        [dt, a_neg[l]], [dta])
                    yield
                    cacc = A("cacc", [128, NTM])
                    for c in range(6):
                        cv = cacc[:, 0:NT].rearrange("p (s t) -> p s t", s=nseg)
                        TSo("dve", cv, sxv(c)[:, :, 0:TT_], cw_t[:, l, c, 0:1], ALU.mult, [sx, cw_t, cb_t], [cacc],
                            s2=cb_t[:, l, c:c + 1], op1=ALU.add)
                        for i in range(1, 4):
                            STT(cv, sxv(c)[:, :, i:i + TT_], cw_t[:, l, c, i:i + 1], cv, ALU.mult, ALU.add, [sx, cw_t, cacc], [cacc])
                        ACT(xbc[:, c, 0:NT], cacc[:, 0:NT], AF.Silu, [cacc], [xbc])
                        yield
                    for g in range(G):
                        tk = slice(g * 128, (g + 1) * 128)
                        xtm = A("xtm", [128, 768], BF16)
                        for half in range(2):
                            pt = PSA()
                            ptb = pt[:].bitcast(BF16)
                            for q in range(3):
                                c = half * 3 + q
                                TR(ptb[:, q * 128:(q + 1) * 128], xbc[:, c, tk], identb[:], [xbc, identb], [pt])
                            if half == 0:
                                ACT(xtm[:, 0:384], ptb[:, 0:384], AF.Copy, [pt], [xtm])
                            else:
                                ACT(xtm[:, 384:768], ptb[:, 0:384], AF.Copy, [pt], [xtm])
                            PSF(pt)
                            yield
                        vs = A("vs", [128, 512], BF16)
                        TTo("pool", vs[:].rearrange("p (h v) -> p h v", h=8), xtm[:, 0:512].rearrange("p (h v) -> p h v", h=8),
                            dt[:, g, :].unsqueeze(2).to_broadcast([128, 8, 64]), ALU.mult, [xtm, dt], [vs])
                        yield
                        pg = PSA()
                        MM(pg[:, 0:8], TRI, dta[:, g, :], True, True, [cst, dta], [pg])
                        MM(pg[:, 8:16], cst[:, C_SEGALL:C_SEGALL + 128], dta[:, g, :], True, True, [cst, dta], [pg])
                        negg = A("negg", [128, 8])
                        ei = A("ei", [128, 8])
                        fj = A("fj", [128, 8])
                        ACT(negg[:], pg[:, 0:8], AF.Copy, [pg], [negg], scale=-1.0)
                        ACT(ei[:], pg[:, 0:8], AF.Exp, [pg], [ei])
                        TTo("dve", fj[:], pg[:, 8:16], negg[:], ALU.add, [pg, negg], [fj])
                        PSF(pg)
                        yield
                        ACT(fj[:], fj[:], AF.Exp, [fj], [fj])
                        dtr = A("dtr", [128, 8, 128])
                        CP("pool", dtr[:], dta[:, g, :].unsqueeze(2).to_broadcast([128, 8, 128]), [dta], [dtr])
                        als = A("als", [128, 4, 16])
                        yield
                        pa = PSA()
                        for h in range(8):
                            MM(pa[:, h * 16:h * 16 + nseg], dtr[:, h, :], cst[:, C_SEGIND:C_SEGIND + nseg], True, True,
                               [dtr, cst], [pa])
                        for hl in range(2):
                            ACT(als[hl * 64:(hl + 1) * 64, :, 0:nseg],
                                pa[hl * 64:(hl + 1) * 64, 0:128].rearrange("p (u x) -> p u x", u=4)[:, :, hl * 16:hl * 16 + nseg],
                                AF.Exp, [pa], [als])
                        PSF(pa)
                        yield
                        Lm = A("Lm", [128, 8, 128])
                        for hh in range(2):
                            pl = PSA()
                            for q in range(4):
                                h = hh * 4 + q
                                MM(pl[:, q * 128:(q + 1) * 128], dtr[:, h, :], TRI, True, False, [dtr, cst], [pl])
                                MM(pl[:, q * 128:(q + 1) * 128], ID, cst[:, C_NEG:C_NEG + 128], False, True, [cst], [pl])
                            for q in range(4):
                                h = hh * 4 + q
                                ACT(Lm[:, h, :], pl[:, q * 128:(q + 1) * 128], AF.Exp, [pl, negg], [Lm], bias=negg[:, h:h + 1])
                            PSF(pl)
                            yield
                        ctm = A("ctm", [128, 2, 128], BF16)
                        for gg in range(2):
                            TSo("pool", ctm[:, gg, :], xbc[:, 5, tk], cst[:, C_HM2 + gg:C_HM2 + gg + 1], ALU.mult, [xbc, cst], [ctm])
                        pcb = PSA()
                        for gg in range(2):
                            MM(pcb[:, gg * 128:(gg + 1) * 128], xbc[:, 4, tk], ctm[:, gg, :], True, True, [xbc, ctm], [pcb])
                        scm8 = A("scm8", [128, 8, 128], BF16)
                        for gg in range(2):
                            TTo("dve", scm8[:, gg * 4:(gg + 1) * 4, :], Lm[:, gg * 4:(gg + 1) * 4, :],
                                pcb[:, gg * 128:(gg + 1) * 128].unsqueeze(1).to_broadcast([128, 4, 128]), ALU.mult, [Lm, pcb], [scm8])
                        PSF(pcb)
                        yield
                        ce = A("ce", [128, 8, 64])
                        for gg in range(2):
                            TTo("pool", ce[:, gg * 4:(gg + 1) * 4, :],
                                xtm[:, 640 + gg * 64:640 + (gg + 1) * 64].unsqueeze(1).to_broadcast([128, 4, 64]),
                                ei[:, gg * 4:(gg + 1) * 4].unsqueeze(2).to_broadcast([128, 4, 64]), ALU.mult, [xtm, ei], [ce])
                        xm = A("xm", [128, 8, 128], BF16)
                        pce = PSA()
                        for u in range(4):
                            TR(pce[:, u * 128:(u + 1) * 128], ce[:, 2 * u:2 * u + 2, :].rearrange("p a b -> p (a b)"), ID, [ce, cst], [pce])
                        ACT(tmpC[:], pce[:], AF.Copy, [pce], [tmpC])
                        PSF(pce)
                        yield
                        for u in range(4):
                            for hl in range(2):
                                TSo("dve", xm[:, 2 * u + hl, :], tmpC[:, u * 128:(u + 1) * 128], cst[:, C_HM2 + hl:C_HM2 + hl + 1], ALU.mult,
                                    [tmpC, cst], [xm])
                        yield
                        Bm = A("Bm", [128, 8, 128], BF16)
                        MSET("pool", Bm[:], 0.0, [Bm])
                        for h in range(8):
                            hl = h % 2
                            gg = h // 4
                            TSo("pool" if h % 2 else "dve", Bm[:, h, hl * 64:(hl + 1) * 64], xtm[:, 512 + gg * 64:512 + (gg + 1) * 64], fj[:, h:h + 1], ALU.mult,
                                [xtm, fj], [Bm])
                        yield
                        po = PSA()
                        pu_ssd = [PSA(), PSA() if nseg > 8 else None]
                        sns = A("sns", [128, 16, 64])
                        for u in range(4):
                            yield
                            if kind == "p":
                                CP("pool", Sbf[:, 3 + u, :], Ss[l][u][:], [Ss[l][u]], [Sbf])
                            else:
                                DMA("sp", S0[:], st_ssd[l][:, 2 * u:2 * u + 2].rearrange("s h k v -> (h k) s v"), [], [S0])
                                CP("dve", Sbf[:], S0[:], [S0], [Sbf])
                            for hl in range(2):
                                h = 2 * u + hl
                                MM(po[:, h * 64:(h + 1) * 64], scm8[:, h, :], vs[:, h * 64:(h + 1) * 64], True, False, [scm8, vs], [po])
                                if kind == "p":
                                    MM(po[:, h * 64:(h + 1) * 64], xm[:, h, :], Sbf[:, 3 + u, :], False, True, [xm, Sbf], [po])
                                else:
                                    CP("dve", QD, xm[:, h, :].rearrange("p (s t) -> p s t", s=16), [xm], [Qms])
                                    for s in range(16):
                                        MM(po[:, h * 64:(h + 1) * 64], Qms[:, s, :], Sbf[:, s, :], False, s == 15, [Qms, Sbf], [po])
                            pu = pu_ssd
                            U_mm(pu, [(Bm[:, 2 * u + hl, :], Bm, vs[:, (2 * u + hl) * 64:(2 * u + hl + 1) * 64], vs) for hl in range(2)])
                            if kind == "p":
                                STT(Ss[l][u][:], Ss[l][u][:], als[:, u, 0:1], pu[0][:, 0:64], ALU.mult, ALU.add,
                                    [Ss[l][u], als, pu[0]], [Ss[l][u]])
                                if last_prompt and g == G - 1:
                                    DMA("sp", o_ssd_p[l, u], Ss[l][u][:], [Ss[l][u]], [])
                                    outs.append(Ss[l][u])
                            else:
                                for hf in range(2):
                                    TTo("dve", tmpA[:].rearrange("p (s v) -> p s v", s=8), S0[:, hf * 8:(hf + 1) * 8, :],
                                        als[:, u, hf * 8:(hf + 1) * 8].unsqueeze(2).to_broadcast([128, 8, 64]), ALU.mult, [S0, als], [tmpA])
                                    TTo("dve", sns[:, hf * 8:(hf + 1) * 8, :], tmpA[:].rearrange("p (s v) -> p s v", s=8),
                                        pu[hf][:].rearrange("p (s v) -> p s v", s=8), ALU.add, [tmpA, pu[hf]], [sns])
                                DMA("sp", o_ssd_s[l, u], sns[:], [sns], [])
                                outs.append(sns)
                        PSF(pu_ssd[0])
                        if pu_ssd[1] is not None:
                            PSF(pu_ssd[1])
                        yield
                        ys = A("ys", [128, 512])
                        TTo("pool", tmpC[:].rearrange("p (h v) -> p h v", h=8), xtm[:, 0:512].rearrange("p (h v) -> p h v", h=8),
                            rows_t[l][:, 1040:1048].unsqueeze(2).to_broadcast([128, 8, 64]), ALU.mult, [xtm, rows_t[l]], [tmpC])
                        TTo("dve", ys[:], tmpC[:], po[:], ALU.add, [tmpC, po], [ys])
                        PSF(po)
                        yield
                        TTo("pool", ys[:], ys[:], szs[g][:], ALU.mult, [ys, szs[g]], [ys])
                        TTo("pool", tmpC[:], ys[:], ys[:], ALU.mult, [ys], [tmpC])
                        S.op("dve", lambda e: e.tensor_reduce(sm[:, 36:37], tmpC[:], AX.X, ALU.add), [tmpC], [sm])
                        TSo("dve", sm[:, 36:37], sm[:, 36:37], 1.0 / 512.0, ALU.mult, [sm], [sm], s2=float(EPS), op1=ALU.add)
                        yield
                        ACT(sm[:, 37:38], sm[:, 36:37], AF.Ln, [sm], [sm])
                        ACT(sm[:, 38:39], sm[:, 37:38], AF.Exp, [sm], [sm], scale=-0.5)
                        STT(ys[:], ys[:], sm[:, 38:39], rows_t[l][:, 512:1024], ALU.mult, ALU.mult, [ys, sm, rows_t[l]], [ys])
                        yield
                        emit_merged(g, ys, 4, 4, [ys])
                        yield

                if KSG <= 3:
                    continue
                if kind == "s":
                    MSET("dve", Qms[:], 0.0, [Qms])
                chains_ = [ssd_chain(), gla_chain(), ret_chain()]
                if ADA_IN_CHAIN and gi == 0 and l == 0:
                    chains_.append(emit_ada(1, PSA, PSF))
                run_chains(chains_, kind == "p")

                if os.environ.get("KDBG") == "1" and kind == "s" and l == 0:
                    dbgt = A("dbgt", [128, 8, 128])
                    CP("dve", dbgt[:], mergedT[:, :, 0:128], mTb[:1], [dbgt])
                    DMA("sp", dbg_out, dbgt[:], [dbgt], [])
                if KSG <= 4:
                    continue
                DMA("sp", lnr[0][:], ln_rows[l, 0].partition_broadcast(128), [], [lnr[0]])
                DMA("sp", lnr[1][:], ln_rows[l, 1].partition_broadcast(128), [], [lnr[1]])
                for hf in range(2):
                    sl = next_slot()
                    for q in range(4):
                        dc = hf * 4 + q
                        pb = PS()
                        for kc in range(8):
                            MM(pb[:, 0:NT], sl[:, kc, q * 128:(q + 1) * 128], mergedT[:, kc, 0:NT], kc == 0, kc == 7,
                               [sl] + mTb[:G], [pb])
                        gate_evac(kind, l, 2, dc, FM[:, dc, 0:NT], pb[:, 0:NT], NT, [pb], [FM])
                for g in range(G):
                    pbs = [PS(), PS()]
                    for dc in range(8):
                        TR(pbs[dc // 4][:, (dc % 4) * 128:(dc % 4 + 1) * 128], FM[:, dc, g * 128:(g + 1) * 128], ID, [FM, cst], [pbs[dc // 4]])
                    layer_norm_tile(g, pbs, lnr[0], lnr[1])

                if stop_after == ("mixer", l):
                    break

                if KSG <= 5:
                    continue
                h32 = [A("h32_%d" % g, [128, 8, 128]) for g in range(GM)]
                make_HT(kind, l, G, 4, 3, h32=h32)
                comb = A("comb", [128, GM, 16])
                combT = View(mergedT, mergedT[0:16, 2:4, :])
                LG = tmpB[:, 0:GM * 20].rearrange("p (g n) -> p g n", g=GM)[:, 0:G, :]
                Rr = tmpB[:, 80:80 + GM * 32].rearrange("p (g n) -> p g n", g=GM)[:, 0:G, :]
                ME = tmpB[:, 208:208 + GM * 64].rearrange("p (g n) -> p g n", g=GM)[:, 0:G, :]
                tB = [tmpB]
                pr = PS()
                for g in range(G):
                    for kc in range(8):
                        MM(pr[:, g * 20:(g + 1) * 20], h32[g][:, kc, :], wrt_t[:, l, kc, :], kc == 0, kc == 7, [h32[g], wrt_t], [pr])
                TTo("dve", LG, pr[:, 0:G * 20].rearrange("p (g n) -> p g n", g=G),
                    brt_t[l][:].unsqueeze(1).to_broadcast([128, G, 20]), ALU.add, [pr, brt_t[l]], tB)

                def RED(out, in_, op):
                    S.op("dve", lambda e: e.tensor_reduce(out, in_, AX.X, op), tB, tB)

                def bc(ap, n):
                    return ap.to_broadcast([128, G, n])

                RED(Rr[:, :, 0], LG[:, :, 0:4], ALU.max)
                TTo("dve", Rr[:, :, 1:5], LG[:, :, 0:4], bc(Rr[:, :, 0:1], 4), ALU.subtract, tB, tB)
                TSo("dve", Rr[:, :, 5:9], Rr[:, :, 1:5], 0.0, ALU.is_ge, tB, tB)
                ACT(Rr[:, :, 9:13], Rr[:, :, 1:5], AF.Exp, tB, tB)
                RED(Rr[:, :, 13], Rr[:, :, 9:13], ALU.add)
                S.op("dve", lambda e, a=Rr[:, :, 14], b=Rr[:, :, 13]: e.reciprocal(a, b), tB, tB)
                TSo("dve", Rr[:, :, 16:20], Rr[:, :, 5:9], 1.0e9, ALU.mult, tB, tB, s2=-1.0e9, op1=ALU.add)
                TTo("dve", ME[:, :, 0:16].rearrange("p g (a e) -> p g a e", a=4), LG[:, :, 4:20].rearrange("p g (a e) -> p g a e", a=4),
                    Rr[:, :, 16:20].unsqueeze(3).to_broadcast([128, G, 4, 4]), ALU.add, tB, tB)
                RED(Rr[:, :, 20], ME[:, :, 0:16], ALU.max)
                TTo("dve", ME[:, :, 16:32], ME[:, :, 0:16], bc(Rr[:, :, 20:21], 16), ALU.is_ge, tB, tB)
                STT(ME[:, :, 32:48], ME[:, :, 16:32], -2.0e9, ME[:, :, 0:16], ALU.mult, ALU.add, tB, tB)
                RED(Rr[:, :, 21], ME[:, :, 32:48], ALU.max)
                TTo("dve", ME[:, :, 48:64], ME[:, :, 32:48], bc(Rr[:, :, 21:22], 16), ALU.is_ge, tB, tB)
                TTo("dve", Rr[:, :, 22:23], Rr[:, :, 21:22], Rr[:, :, 20:21], ALU.subtract, tB, tB)
                ACT(Rr[:, :, 23:24], Rr[:, :, 22:23], AF.Exp, tB, tB)
                TSo("dve", Rr[:, :, 24:25], Rr[:, :, 23:24], 1.0, ALU.add, tB, tB)
                S.op("dve", lambda e, a=Rr[:, :, 25:26], b=Rr[:, :, 24:25]: e.reciprocal(a, b), tB, tB)
                TTo("dve", Rr[:, :, 26:27], Rr[:, :, 23:24], Rr[:, :, 25:26], ALU.mult, tB, tB)
                TTo("dve", Rr[:, :, 25:27], Rr[:, :, 25:27], bc(Rr[:, :, 14:15], 2), ALU.mult, tB, tB)
                TTo("dve", comb[:, 0:G, :], ME[:, :, 16:32], bc(Rr[:, :, 25:26], 16), ALU.mult, tB, [comb])
                TTo("dve", ME[:, :, 0:16], ME[:, :, 48:64], bc(Rr[:, :, 26:27], 16), ALU.mult, tB, tB)
                TTo("dve", comb[:, 0:G, :], comb[:, 0:G, :], ME[:, :, 0:16], ALU.add, [comb, tmpB], [comb])
                pc = PS()
                for g in range(G):
                    TR(pc[0:16, g * 128:(g + 1) * 128], comb[:, g, :], ID, [comb, cst], [pc])
                CP("dve", combT[:, 0, 0:NT], pc[0:16, 0:NT], [pc], [combT])
                c32 = FM[0:16, 0, 0:NT]
                CP("dve", c32, combT[:, 0, 0:NT], [combT], [FM])
                TTo("dve", combT[:, 1, 0:NT], pc[0:16, 0:NT], c32, ALU.subtract, [pc, FM], [combT])
                if os.environ.get("KDBG") == "2" and gi == 0 and l == 0:
                    DMA("sp", dbg_out[:, 0, 0:GM * 16], comb[:].rearrange("p a b -> p (a b)"), [comb], [])
                if KSG <= 6:
                    continue
                sel = A("sel", [16, 16, 128], BF16)
                CP("dve", sel[:], cst[0:16, C_ID:C_ID + 16].unsqueeze(2).to_broadcast([16, 16, 128]), [cst], [sel])
                hidT = View(mergedT, mergedT[:, 0:2, :])
                hidTb = [Buf("hid_fc0"), Buf("hid_fc1")]
                sact = A("sact", [128, NTM])
                tact = A("tact", [128, NTM])
                for ex in range(16):
                    sl13 = next_slot()
                    sl2 = next_slot()
                    w2v = w2view(sl2)
                    pbc = PS()
                    MM(pbc[:, 0:NT], sel[:, ex, :], combT[:, 0, 0:NT], True, False, [sel, combT], [pbc])
                    MM(pbc[:, 0:NT], sel[:, ex, :], combT[:, 1, 0:NT], False, True, [sel, combT], [pbc])
                    for fc in range(2):
                        p1 = PS()
                        p3 = PS()
                        for kc in range(8):
                            MM(p1[:, 0:NT], sl13[:, kc, fc * 128:(fc + 1) * 128], HT[:, kc, 0:NT], kc == 0, kc == 7, [sl13] + HTb[:G], [p1])
                        for kc in range(8):
                            MM(p3[:, 0:NT], sl13[:, kc, 256 + fc * 128:256 + (fc + 1) * 128], HT[:, kc, 0:NT], kc == 0, kc == 7,
                               [sl13] + HTb[:G], [p3])
                        ACT(sact[:, 0:NT], p1[:, 0:NT], AF.Silu, [p1], [sact])
                        TTo("dve", tact[:, 0:NT], sact[:, 0:NT], p3[:, 0:NT], ALU.mult, [sact, p3], [tact])
                        TTo("dve", hidT[:, fc, 0:NT], tact[:, 0:NT], pbc[:, 0:NT], ALU.mult, [tact, pbc], [hidTb[fc]])
                    for dh in range(2):
                        pys = [PS() for _ in range(4)]
                        for fc in range(2):
                            for q in range(4):
                                dc = dh * 4 + q
                                MM(pys[q][:, 0:NT], w2v[:, fc, dc * 128:(dc + 1) * 128], hidT[:, fc, 0:NT], fc == 0, fc == 1,
                                   [sl2, hidTb[fc]], [pys[q]])
                        for q in range(4):
                            dc = dh * 4 + q
                            if ex == 0:
                                ACT(FM[:, dc, 0:NT], pys[q][:, 0:NT], AF.Copy, [pys[q]], [FM])
                            else:
                                TTo("dve", FM[:, dc, 0:NT], FM[:, dc, 0:NT], pys[q][:, 0:NT], ALU.add, [FM, pys[q]], [FM])
                DMA("sp", lnr[0][:], ln_rows[l, 2].partition_broadcast(128), [], [lnr[0]])
                DMA("sp", lnr[1][:], ln_rows[l, 3].partition_broadcast(128), [], [lnr[1]])
                for dc in range(8):
                    gate_evac(kind, l, 5, dc, FM[:, dc, 0:NT], FM[:, dc, 0:NT], NT, [FM], [FM])
                for g in range(G):
                    pbs = [PS(), PS()]
                    for dc in range(8):
                        TR(pbs[dc // 4][:, (dc % 4) * 128:(dc % 4 + 1) * 128], FM[:, dc, g * 128:(g + 1) * 128], ID, [FM, cst], [pbs[dc // 4]])
                    layer_norm_tile(g, pbs, lnr[0], lnr[1])
                if stop_after == ("moe", l):
                    break

            for g in range(G):
                yb = Buf("y%d" % tiles[g])
                DMA("sp", y_all[tiles[g]], X[g][:], [X[g]], [yb])
                outs.append(yb)

        d = S.dq["sp"]
        fin = []
        for i, sem in enumerate(d["sems"]):
            if d["cnt"][i] > 0:
                fin.append((sem, d["cnt"][i] * 16))
        S.lists["sp"].append((fin, None, None, 0))
        with nc.Block() as block:
            S.emit(block)
    return nc, S


_CACHE = {}


def prep_inputs(inp):
    f = lambda a: np.ascontiguousarray(np.asarray(a, dtype=np.float32))
    shared = {}
    shared["consts_p"] = make_consts("p")
    shared["consts_s"] = make_consts("s")
    shared["trig"] = make_trig()
    shared["w_ada"] = f(inp["w_ada"])
    shared["b_adaT"] = f(np.asarray(inp["b_ada"]).reshape(2, 48, 128).transpose(2, 0, 1))
    shared["w_in"] = f(inp["w_in"])
    shared["w_gate"] = f(np.asarray(inp["gla_w_gate"]).transpose(1, 0, 2))
    shared["b_gateT"] = f(np.asarray(inp["gla_b_gate"]).T)
    shared["rows"] = f(np.concatenate([np.asarray(inp[k]) for k in
                                       ("gla_norm", "ret_norm", "ssd_norm", "ssd_dt_bias", "ssd_a_log", "ssd_d")], axis=1)[:, None, :])
    shared["conv_wT"] = f(np.asarray(inp["ssd_conv_w"]).reshape(2, 4, 6, 128).transpose(3, 0, 2, 1))
    shared["conv_bT"] = f(np.asarray(inp["ssd_conv_b"]).reshape(2, 6, 128).transpose(2, 0, 1))
    shared["w_out"] = f(inp["w_out"])
    shared["ln_rows"] = f(np.stack([np.asarray(inp[k]) for k in ("ln1_g", "ln1_b", "ln2_g", "ln2_b")], axis=1)[:, :, None, :])
    wr = np.concatenate([np.asarray(inp["moe_w_group"]), np.asarray(inp["moe_w_expert"])], axis=2)
    shared["w_rt"] = f(wr.reshape(2, 8, 128, 20).transpose(2, 0, 1, 3))
    shared["b_rt"] = f(np.concatenate([np.asarray(inp["moe_b_group"]), np.asarray(inp["moe_b_expert"])], axis=1)[:, None, :])
    shared["moe_w1"] = f(inp["moe_w1"])
    shared["moe_w3"] = f(inp["moe_w3"])
    shared["moe_w2"] = f(inp["moe_w2"])
    xp = np.asarray(inp["x_prompt"], dtype=np.float32)
    xs = np.asarray(inp["x_sample"], dtype=np.float32)
    cp = np.asarray(inp["c_prompt"], dtype=np.float32)
    cs = np.asarray(inp["c_sample"], dtype=np.float32)
    maps = []
    for c in range(NCORE):
        m = dict(shared)
        m["x_all"] = f(np.concatenate([xp[c].reshape(16, 128, D), xs[16 * c:16 * c + 16].reshape(1, 128, D)], axis=0))
        cc = np.zeros((18, D), np.float32)
        cc[0] = cp[c]
        cc[1:17] = cs[16 * c:16 * c + 16]
        m["cT"] = f(cc.reshape(18, 8, 128).transpose(2, 1, 0))
        m["st_gla"] = f(np.asarray(inp["state_gla"])[:, 16 * c:16 * c + 16])
        m["st_ret"] = f(np.asarray(inp["state_ret"])[:, 16 * c:16 * c + 16])
        m["st_ssd"] = f(np.asarray(inp["state_ssd"])[:, 16 * c:16 * c + 16])
        sc = np.asarray(inp["state_conv"])[:, 16 * c:16 * c + 16]
        m["st_conv"] = f(sc.reshape(2, 16, 3, 6, 128).transpose(0, 4, 3, 1, 2))
        maps.append(m)
    return maps


def assemble(results):
    y_prompt = np.zeros((8, 2048, D), np.float32)
    y_sample = np.zeros((128, 8, D), np.float32)
    gla_p = np.zeros((2, 8, 4, 32, 64), np.float32)
    ret_p = np.zeros((2, 8, 4, 64, 64), np.float32)
    ssd_p = np.zeros((2, 8, 8, 64, 64), np.float32)
    conv_p = np.zeros((2, 8, 3, 768), np.float32)
    gla_s = np.zeros((2, 128, 4, 32, 64), np.float32)
    ret_s = np.zeros((2, 128, 4, 64, 64), np.float32)
    ssd_s = np.zeros((2, 128, 8, 64, 64), np.float32)
    conv_s = np.zeros((2, 128, 3, 768), np.float32)
    for c, r in enumerate(results):
        y = r["y_all"]
        y_prompt[c] = y[:16].reshape(2048, D)
        y_sample[16 * c:16 * c + 16] = y[16].reshape(16, 8, D)
        gla_p[:, c] = r["o_gla_p"].reshape(2, 4, 32, 64)
        ret_p[:, c] = r["o_ret_p"].reshape(2, 2, 2, 64, 64).reshape(2, 4, 64, 64)
        ssd_p[:, c] = r["o_ssd_p"].reshape(2, 4, 2, 64, 64).reshape(2, 8, 64, 64)
        conv_p[:, c] = r["o_conv_p"]
        gla_s[:, 16 * c:16 * c + 16] = r["o_gla_s"].reshape(2, 4, 32, 16, 64).transpose(0, 3, 1, 2, 4)
        ret_s[:, 16 * c:16 * c + 16] = r["o_ret_s"].reshape(2, 2, 2, 64, 16, 64).transpose(0, 4, 1, 2, 3, 5).reshape(2, 16, 4, 64, 64)
        ssd_s[:, 16 * c:16 * c + 16] = r["o_ssd_s"].reshape(2, 4, 2, 64, 16, 64).transpose(0, 4, 1, 2, 3, 5).reshape(2, 16, 8, 64, 64)
        conv_s[:, 16 * c:16 * c + 16] = r["o_conv_s"].reshape(2, 16, 3, 768)
    return (y_prompt, y_sample, gla_p, ret_p, ssd_p, conv_p, gla_s, ret_s, ssd_s, conv_s)


def kernel(**inputs):
    if "nc" not in _CACHE:
        _CACHE["nc"] = build()[0]
    nc = _CACHE["nc"]
    maps = prep_inputs(inputs)
    res = run_bass_kernel_spmd(nc, maps, core_ids=list(range(NCORE)))
    return assemble(res.results)
```

```python
import os
import numpy as np
from contextlib import ExitStack
import concourse.bass as bass
import concourse.mybir as mybir
from concourse.bass_utils import run_bass_kernel_spmd

F32 = mybir.dt.float32
BF16 = mybir.dt.bfloat16
AF = mybir.ActivationFunctionType
ALU = mybir.AluOpType
AX = mybir.AxisListType

D = 1024
DEPTH = 2
NCORE = 8
PAST_LEN = 16384
ALPHA = (2 * DEPTH) ** 0.25
EPS = 1e-5
N_IN = 3096
NEG = -1.0e5
C_ID, C_TRI, C_NEG, C_DM, C_GQ, C_GK, C_AL, C_HM4, C_HM2, C_SEGALL, C_SEGIND, C_END = (
    0, 128, 256, 384, 896, 1152, 1156, 1158, 1162, 1164, 1292, 1308)


class Buf:
    __slots__ = ("name", "last_w", "readers")

    def __init__(self, name):
        self.name = name
        self.last_w = None
        self.readers = {}


class T:
    def __init__(self, h, name):
        self.h = h
        self.b = Buf(name)

    def __getitem__(self, k):
        return self.h[k]


class View:
    def __init__(self, parent, ap):
        self.h = ap
        self.b = parent.b

    def __getitem__(self, k):
        return self.h[k]


def _b(x):
    return x.b if isinstance(x, (T, View)) else x


class Sched:
    ENG = ("pe", "act", "dve", "pool", "sp")

    def __init__(self, nc, stack, n_dma_sems=8):
        self.nc = nc
        self.lists = {e: [] for e in self.ENG}
        self.sem = {e: stack.enter_context(nc.semaphore("s_" + e)) for e in ("pe", "act", "dve", "pool")}
        self.cnt = {e: 0 for e in ("pe", "act", "dve", "pool")}
        self.dq = {}
        for q in ("sp", "pool"):
            sems = [stack.enter_context(nc.semaphore("d_%s%d" % (q, i))) for i in range(n_dma_sems)]
            self.dq[q] = {"sems": sems, "cnt": [0] * n_dma_sems, "next": 0}
        self.waited = {e: {} for e in self.ENG}
        self.n = 0

    def _deps(self, reads, writes):
        deps = []
        for b in reads:
            if b.last_w is not None:
                deps.append(b.last_w)
        for b in writes:
            if b.last_w is not None:
                deps.append(b.last_w)
            deps.extend(b.readers.values())
        return deps

    def _waits(self, e, deps):
        waits = []
        for (key, sem, val, eng) in deps:
            if eng == "pe" and e == "pe":
                continue
            if self.waited[e].get(key, 0) >= val:
                continue
            self.waited[e][key] = val
            waits.append((sem, val))
        return waits

    def op(self, e, fn, r=(), w=()):
        reads = [_b(x) for x in r]
        writes = [_b(x) for x in w]
        waits = self._waits(e, self._deps(reads, writes))
        self.cnt[e] += 1
        self.n += 1
        self.lists[e].append((waits, fn, self.sem[e], 1))
        tok = (e, self.sem[e], self.cnt[e], e)
        for b in reads:
            b.readers[e] = tok
        for b in writes:
            b.last_w = tok
            b.readers = {}

    def dma(self, q, fn, r=(), w=()):
        reads = [_b(x) for x in r]
        writes = [_b(x) for x in w]
        d = self.dq[q]
        i = d["next"]
        d["next"] = (i + 1) % len(d["sems"])
        sem = d["sems"][i]
        deps = self._deps(reads, writes)
        key = "d_%s%d" % (q, i)
        if d["cnt"][i] > 0:
            deps.append((key, sem, d["cnt"][i] * 16, "dma"))
        waits = self._waits(q, deps)
        d["cnt"][i] += 1
        self.n += 1
        self.lists[q].append((waits, fn, sem, 16))
        tok = (key, sem, d["cnt"][i] * 16, "dma")
        for b in reads:
            b.readers[key] = tok
        for b in writes:
            b.last_w = tok
            b.readers = {}

    def final_wait(self, q, bufs):
        deps = []
        for b in bufs:
            b = _b(b)
            if b.last_w is not None:
                deps.append(b.last_w)
        self.lists[q].append((self._waits(q, deps), None, None, 0))

    def emit(self, block):
        def run(eng, lst):
            for waits, fn, sem, inc in lst:
                for (s, v) in waits:
                    eng.wait_ge(s, v)
                if fn is not None:
                    fn(eng).then_inc(sem, inc)

        @block.tensor
        def _(eng):
            run(eng, self.lists["pe"])

        @block.scalar
        def _(eng):
            run(eng, self.lists["act"])

        @block.vector
        def _(eng):
            run(eng, self.lists["dve"])

        @block.gpsimd
        def _(eng):
            run(eng, self.lists["pool"])

        @block.sync
        def _(eng):
            run(eng, self.lists["sp"])


def make_consts(kind):
    p = np.arange(128)
    if kind == "p":
        seg = np.zeros(128, np.int64)
        pos = p.copy()
        L = 128
        nseg = 1
    else:
        seg = p // 8
        pos = p % 8
        L = 8
        nseg = 16
    same = seg[:, None] == seg[None, :]
    c = np.zeros((128, C_END), np.float32)
    c[:, C_ID:C_ID + 128] = np.eye(128)
    caus = (p[:, None] <= p[None, :]) & same
    c[:, C_TRI:C_TRI + 128] = caus
    c[:, C_NEG:C_NEG + 128] = np.where(caus, 0.0, NEG)
    gam = 1.0 - 2.0 ** (-5.0 - np.arange(4, dtype=np.float64))
    lg = np.log(gam)
    for h in range(4):
        dm = np.where(caus, np.exp(lg[h] * (pos[None, :] - pos[:, None]).astype(np.float64)), 0.0)
        c[:, C_DM + h * 128:C_DM + (h + 1) * 128] = dm
        c[:, C_GK + h] = np.exp(lg[h] * (L - 1 - pos))
    for u in range(2):
        for hl in range(2):
            h = 2 * u + hl
            c[hl * 64:(hl + 1) * 64, C_GQ + u * 128:C_GQ + (u + 1) * 128] = np.exp(lg[h] * (pos[None, :] + 1.0))
            c[hl * 64:(hl + 1) * 64, C_AL + u] = np.exp(lg[h] * L)
    for h in range(4):
        c[:, C_HM4 + h] = (p // 32 == h)
    for h in range(2):
        c[:, C_HM2 + h] = (p // 64 == h)
    c[:, C_SEGALL:C_SEGALL + 128] = same
    for s in range(nseg):
        c[:, C_SEGIND + s] = (seg == s)
    return c


def make_trig():
    half = 32
    inv = (np.float32(10000.0) ** (-np.arange(half, dtype=np.float32) / np.float32(half))).astype(np.float32)
    out = np.zeros((17, 128, 128), np.float32)
    for t in range(17):
        if t < 16:
            pos = (t * 128 + np.arange(128)).astype(np.float32)
        else:
            pos = (PAST_LEN + (np.arange(128) % 8)).astype(np.float32)
        ang = (pos[:, None] * inv[None, :]).astype(np.float32)
        co = np.cos(ang).astype(np.float32)
        si = np.sin(ang).astype(np.float32)
        out[t, :, 0:32] = co
        out[t, :, 32:64] = co
        out[t, :, 64:96] = -si
        out[t, :, 96:128] = si
    return out


def build(n_layers=DEPTH, groups=None, stop_after=None):
    nc = bass.Bass("TRN2", target_bir_lowering=False)
    if groups is None:
        groups = [("p", [0, 1, 2, 3]), ("p", [4, 5, 6, 7]), ("p", [8, 9, 10, 11]), ("p", [12, 13, 14, 15]), ("s", [16])]
    GM = max(len(g[1]) for g in groups)
    NTM = GM * 128

    def din(name, shape, dt=F32):
        return nc.dram_tensor(name, list(shape), dt, kind="ExternalInput").ap()

    def dout(name, shape, dt=F32):
        return nc.dram_tensor(name, list(shape), dt, kind="ExternalOutput").ap()

    x_all = din("x_all", [17, 128, D])
    cT = din("cT", [128, 8, 18])
    consts_p = din("consts_p", [128, C_END])
    consts_s = din("consts_s", [128, C_END])
    trig = din("trig", [17, 128, 128])
    st_gla = din("st_gla", [2, 16, 4, 32, 64])
    st_ret = din("st_ret", [2, 16, 4, 64, 64])
    st_ssd = din("st_ssd", [2, 16, 8, 64, 64])
    st_conv = din("st_conv", [2, 128, 6, 16, 3])
    w_ada = din("w_ada", [2, D, 6 * D])
    b_adaT = din("b_adaT", [128, 2, 48])
    w_in = din("w_in", [2, D, N_IN])
    w_gate = din("w_gate", [16, 2, 128])
    b_gateT = din("b_gateT", [128, 2])
    rows = din("rows", [2, 1, 1048])
    conv_wT = din("conv_wT", [128, 2, 6, 4])
    conv_bT = din("conv_bT", [128, 2, 6])
    w_out = din("w_out", [2, D, D])
    ln_rows = din("ln_rows", [2, 4, 1, D])
    w_rt = din("w_rt", [128, 2, 8, 20])
    b_rt = din("b_rt", [2, 1, 20])
    moe_w1 = din("moe_w1", [2, 16, D, 256])
    moe_w3 = din("moe_w3", [2, 16, D, 256])
    moe_w2 = din("moe_w2", [2, 16, 256, D])

    y_all = dout("y_all", [17, 128, D])
    o_gla_p = dout("o_gla_p", [2, 128, 64])
    o_ret_p = dout("o_ret_p", [2, 2, 128, 64])
    o_ssd_p = dout("o_ssd_p", [2, 4, 128, 64])
    o_conv_p = dout("o_conv_p", [2, 3, 768])
    o_gla_s = dout("o_gla_s", [2, 128, 16, 64])
    o_ret_s = dout("o_ret_s", [2, 2, 128, 16, 64])
    o_ssd_s = dout("o_ssd_s", [2, 4, 128, 16, 64])
    o_conv_s = dout("o_conv_s", [2, 48, 768])
    outs = []
    if os.environ.get("KDBG") in ("1", "2"):
        dbg_out = dout("dbg_out", [128, 8, 128])

    with ExitStack() as st:
        S = Sched(nc, st)
        _cnt = [0]

        def sb(shape, dt=F32, name=None):
            _cnt[0] += 1
            nm = "%s_%d" % (name or "t", _cnt[0])
            return T(st.enter_context(nc.sbuf_tensor(nm, list(shape), dt)), nm)

        def MM(out, lhsT, rhs, start, stop, r, w):
            S.op("pe", lambda e: e.matmul(out, lhsT, rhs, start=start, stop=stop), r, w)

        def TR(out, in_, ident, r, w):
            S.op("pe", lambda e: e.transpose(out, in_, ident), r, w)

        def ACT(out, in_, func, r, w, bias=None, scale=None):
            kw = {}
            if bias is not None:
                kw["bias"] = bias
            if scale is not None:
                kw["scale"] = scale
            S.op("act", lambda e: e.activation(out=out, in_=in_, func=func, **kw), r, w)

        def TTo(eng, out, a, b, op, r, w):
            S.op(eng, lambda e: e.tensor_tensor(out=out, in0=a, in1=b, op=op), r, w)

        def TSo(eng, out, a, s1, op0, r, w, s2=None, op1=None):
            if op1 is None:
                S.op(eng, lambda e: e.tensor_scalar(out, a, s1, None, op0), r, w)
            else:
                S.op(eng, lambda e: e.tensor_scalar(out, a, s1, s2, op0, op1), r, w)

        def STT(out, a, scalar, b, op0, op1, r, w):
            S.op("dve", lambda e: e.scalar_tensor_tensor(out=out, in0=a, scalar=scalar, in1=b, op0=op0, op1=op1), r, w)

        def CP(eng, out, in_, r, w):
            S.op(eng, lambda e: e.tensor_copy(out=out, in_=in_), r, w)

        def MSET(eng, ap, val, w):
            S.op(eng, lambda e: e.memset(ap, val), (), w)

        def DMA(q, out, in_, r, w):
            S.dma(q, lambda e: e.dma_start(out=out, in_=in_), r, w)

        banks = []
        for i in range(8):
            banks.append(T(st.enter_context(nc.psum_tensor("bank%d" % i, [128, 512], F32)), "bank%d" % i))
        _bk = [0]

        _free = []
        _mode = [False]

        def PSA():
            assert _free, "out of PSUM banks in chain phase"
            return _free.pop(0)

        def PSF(b):
            _free.append(b)

        def run_chains(gens, interleave):
            _free[:] = [banks[(_bk[0] + i) % 8] for i in range(8)]
            if not interleave:
                for gch in gens:
                    for _ in gch:
                        pass
            else:
                active = list(gens)
                wts = {id(gch): w for gch, w in zip(gens, (1.0, 1.0, 1.5, 0.3))}
                cred = {id(gch): 0.0 for gch in gens}
                while active:
                    for gch in list(active):
                        cred[id(gch)] += wts[id(gch)]
                        while cred[id(gch)] >= 1.0 and gch in active:
                            cred[id(gch)] -= 1.0
                            try:
                                next(gch)
                            except StopIteration:
                                active.remove(gch)
            assert len(_free) == 8, len(_free)

        def PS():
            b = banks[_bk[0] % 8]
            _bk[0] += 1
            return b

        cst = sb([128, C_END], name="cst")
        identb = sb([128, 128], BF16, name="identb")
        trig_t = sb([128, GM, 128], name="trig")
        cT_t = sb([128, 8, 18], name="cT")
        scT = sb([128, 8, 18], BF16, name="scT")
        mod = [sb([128, 48, 18], name="mod%d" % l) for l in range(2)]
        badaT = sb([128, 2, 48], name="badaT")
        wg_t = sb([16, 2, 128], name="wg")
        nbg_t = sb([128, 2], name="nbg")
        rows_1 = sb([128, 1048], name="rows")
        rows_t = [rows_1, rows_1]
        a_neg_1 = sb([128, 8], name="aneg")
        a_neg = [a_neg_1, a_neg_1]
        cw_t = sb([128, 2, 6, 4], name="cw")
        cb_t = sb([128, 2, 6], name="cb")
        wrt_t = sb([128, 2, 8, 20], name="wrt")
        brt_t = [sb([128, 20], name="brt%d" % l) for l in range(2)]
        Pb = [sb([128, 512], name="P%d" % i) for i in range(4)]
        Qb = [sb([128, 1024], name="Q%d" % i) for i in range(4)]
        lnr = [Qb[0], Qb[1]]
        snew = sb([128, 16, 64], name="snew")
        Sg = [sb([128, 64], name="Sg%d" % l) for l in range(2)]
        Sr = [[sb([128, 64], name="Sr%d_%d" % (l, u)) for u in range(2)] for l in range(2)]
        Ss = [[sb([128, 64], name="Ss%d_%d" % (l, u)) for u in range(4)] for l in range(2)]
        chist = [sb([128, 6, 3], name="chist%d" % l) for l in range(2)]
        X = [sb([128, D], name="X%d" % g) for g in range(GM)]
        S0 = View(X[1], X[1][:].rearrange("p (s v) -> p s v", s=16)) if GM > 1 else sb([128, 16, 64], name="S0")
        HT = sb([128, 8, NTM], BF16, name="HT")
        HTb = [Buf("HT%d" % g) for g in range(GM)]
        FM = sb([128, 8, NTM], name="FMacc")
        NSLOT = 5
        slots = [sb([128, 8, 512], BF16, name="slot%d" % i) for i in range(NSLOT)]
        _sl = [0]

        plan = []
        _wi = [0, 0]
        PF = 3

        def kcv(ap2d):
            return ap2d.rearrange("(kc p) n -> p kc n", p=128)

        def plan_ada(l):
            return [[(slice(0, 512), kcv(w_ada[l])[:, :, j * 512:(j + 1) * 512])] for j in range(12)]

        def plan_layer(l, with_ada=None):
            p = []
            for (c0, c1) in ((0, 512), (512, 784), (784, 1296), (1296, 1808), (1808, 2320), (2320, 2832), (2832, 3096)):
                p.append([(slice(0, c1 - c0), kcv(w_in[l])[:, :, c0:c1])])
            if with_ada is not None:
                p.extend(plan_ada(with_ada))
            for hf in range(2):
                p.append([(slice(0, 512), kcv(w_out[l])[:, :, hf * 512:(hf + 1) * 512])])
            return p

        def plan_moe(l):
            p = []
            for ex in range(16):
                p.append([(slice(0, 256), kcv(moe_w1[l, ex])), (slice(256, 512), kcv(moe_w3[l, ex]))])
                p.append([("w2", moe_w2[l, ex].rearrange("(fc p) n -> p fc n", p=128))])
            return p

        plan.extend(plan_ada(0))
        ADA_IN_CHAIN = (n_layers == 2 and groups[0][0] == "p")
        if not ADA_IN_CHAIN:
            for l in range(1, n_layers):
                plan.extend(plan_ada(l))
        for gi_, (kind_, tiles_) in enumerate(groups):
            for l in range(n_layers):
                plan.extend(plan_layer(l, with_ada=(1 if (ADA_IN_CHAIN and gi_ == 0 and l == 0) else None)))
                if stop_after != ("mixer", l):
                    plan.extend(plan_moe(l))
                if stop_after is not None and stop_after[1] == l:
                    break

        def w2view(sl):
            return sl[:].rearrange("p a b -> p (a b)")[:, 0:2048].rearrange("p (a b) -> p a b", a=2)

        def next_slot():
            while _wi[1] < len(plan) and _wi[1] <= _wi[0] + PF:
                i = _wi[1]
                sl = slots[i % NSLOT]
                for (cs, src) in plan[i]:
                    if isinstance(cs, str):
                        DMA("pool", w2view(sl), src, [], [sl])
                    else:
                        DMA("pool", sl[:, :, cs], src, [], [sl])
                _wi[1] += 1
            sl = slots[_wi[0] % NSLOT]
            _wi[0] += 1
            return sl

        ID = cst[:, C_ID:C_ID + 128]

        DMA("sp", cst[:], consts_p, [], [cst])
        DMA("sp", cT_t[:], cT, [], [cT_t])
        DMA("sp", badaT[:], b_adaT, [], [badaT])
        DMA("sp", wg_t[:], w_gate, [], [wg_t])
        DMA("sp", nbg_t[:], b_gateT, [], [nbg_t])
        DMA("sp", cw_t[:], conv_wT, [], [cw_t])
        DMA("sp", cb_t[:], conv_bT, [], [cb_t])
        DMA("sp", wrt_t[:], w_rt, [], [wrt_t])
        for l in range(2):
            DMA("sp", brt_t[l][:], b_rt[l].partition_broadcast(128), [], [brt_t[l]])
        CP("dve", identb[:], ID, [cst], [identb])
        TSo("dve", nbg_t[:], nbg_t[:], -1.0, ALU.mult, [nbg_t], [nbg_t])
        for l in range(2):
            for t_ in [Sg[l]] + Sr[l] + Ss[l] + [chist[l]]:
                MSET("dve", t_[:], 0.0, [t_])

        ACT(scT[:], cT_t[:], AF.Silu, [cT_t], [scT])

        def emit_ada(l, alloc, free):
            for j in range(12):
                sl = next_slot()
                pb = alloc()
                for mc in range(4):
                    for kc in range(8):
                        MM(pb[:, mc * 18:(mc + 1) * 18], sl[:, kc, mc * 128:(mc + 1) * 128], scT[:, kc, :],
                           kc == 0, kc == 7, [sl, scT], [pb])
                TTo("dve", mod[l][:, j * 4:(j + 1) * 4, :], pb[:, 0:72].rearrange("p (a b) -> p a b", a=4),
                    badaT[:, l, j * 4:(j + 1) * 4].unsqueeze(2).to_broadcast([128, 4, 18]), ALU.add,
                    [pb, badaT], [mod[l]])
                free(pb)
                yield
            for comp in (1, 4):
                TSo("dve", mod[l][:, comp * 8:(comp + 1) * 8, :], mod[l][:, comp * 8:(comp + 1) * 8, :], 1.0, ALU.add,
                    [mod[l]], [mod[l]])

        for _ in emit_ada(0, PS, lambda b: None):
            pass
        if not ADA_IN_CHAIN:
            for l_ in range(1, n_layers):
                for _ in emit_ada(l_, PS, lambda b: None):
                    pass

        def w_in_chunk(l, c0, c1):
            return next_slot()

        def mod_evac(kind, l, sc, sh, kc, out_ap, ps_ap, r, w, tmp):
            if kind == "p":
                ACT(out_ap, ps_ap, AF.Identity, r + [mod[l]], w, bias=mod[l][:, sh * 8 + kc, 0:1], scale=mod[l][:, sc * 8 + kc, 0:1])
            else:
                TTo("dve", tmp[:, 0:128].rearrange("p (s t) -> p s t", s=16), ps_ap.rearrange("p (s t) -> p s t", s=16),
                    mod[l][:, sc * 8 + kc, 1:17].unsqueeze(2).to_broadcast([128, 16, 8]), ALU.mult, r + [mod[l]], [tmp])
                TTo("dve", out_ap.rearrange("p (s t) -> p s t", s=16), tmp[:, 0:128].rearrange("p (s t) -> p s t", s=16),
                    mod[l][:, sh * 8 + kc, 1:17].unsqueeze(2).to_broadcast([128, 16, 8]), ALU.add, [tmp, mod[l]], w)

        def gate_evac(kind, l, gc, kc, out_ap, in_ap, ntok, r, w):
            if kind == "p":
                ACT(out_ap, in_ap, AF.Copy, r + [mod[l]], w, scale=mod[l][:, gc * 8 + kc, 0:1])
            else:
                TTo("dve", out_ap.rearrange("p (s t) -> p s t", s=16), in_ap.rearrange("p (s t) -> p s t", s=16),
                    mod[l][:, gc * 8 + kc, 1:17].unsqueeze(2).to_broadcast([128, 16, 8]), ALU.mult, r + [mod[l]], w)

        tmpA = sb([128, 512], name="tmpA")
        tmpB = sb([128, 512], name="tmpB")
        tmpC = sb([128, 512], name="tmpC")
        sm = sb([128, 64], name="small")
        stats = sb([128, 2, 6], name="bnst")

        def make_HT(kind, l, G, sc, sh, h32=None):
            for g in range(G):
                for half in range(2):
                    pb = PS()
                    for q in range(4):
                        kc = half * 4 + q
                        TR(pb[:, q * 128:(q + 1) * 128], X[g][:, kc * 128:(kc + 1) * 128], ID, [X[g], cst], [pb])
                    for q in range(4):
                        kc = half * 4 + q
                        if h32 is None:
                            mod_evac(kind, l, sc, sh, kc, HT[:, kc, g * 128:(g + 1) * 128], pb[:, q * 128:(q + 1) * 128],
                                     [pb], [HTb[g]], tmpA)
                        else:
                            mod_evac(kind, l, sc, sh, kc, h32[g][:, kc, :], pb[:, q * 128:(q + 1) * 128],
                                     [pb], [h32[g]], tmpA)
                            if kind == "p":
                                mod_evac(kind, l, sc, sh, kc, HT[:, kc, g * 128:(g + 1) * 128], pb[:, q * 128:(q + 1) * 128],
                                         [pb], [HTb[g]], tmpA)
                            else:
                                CP("dve", HT[:, kc, g * 128:(g + 1) * 128], h32[g][:, kc, :], [h32[g]], [HTb[g]])

        def fm_proj(sl, col0, ncol, NT, G, evac):
            pb = PS()
            for kc in range(8):
                MM(pb[0:ncol, 0:NT], sl[:, kc, col0:col0 + ncol], HT[:, kc, 0:NT], kc == 0, kc == 7,
                   [sl] + HTb[:G], [pb])
            evac(pb)

        def tm_proj(sl, col0, ncol, g, evac):
            pb = PS()
            for kc in range(8):
                MM(pb[:, 0:ncol], HT[:, kc, g * 128:(g + 1) * 128], sl[:, kc, col0:col0 + ncol], kc == 0, kc == 7,
                   [sl, HTb[g]], [pb])
            evac(pb)

        def layer_norm_tile(g, pbs, lg, lb):
            u = tmpU
            for hf in range(2):
                STT(u[:, hf * 512:(hf + 1) * 512], X[g][:, hf * 512:(hf + 1) * 512], float(ALPHA), pbs[hf][:, 0:512],
                    ALU.mult, ALU.add, [X[g], pbs[hf]], [u])
            for hf in range(2):
                S.op("dve", lambda e, hf=hf: e.bn_stats(stats[:, hf, :], u[:, hf * 512:(hf + 1) * 512]), [u], [stats])
            S.op("dve", lambda e: e.bn_aggr(sm[:, 0:2], stats[:].rearrange("p a b -> p (a b)")), [stats], [sm])
            ACT(sm[:, 3:4], sm[:, 1:2], AF.Ln, [sm, epsc], [sm], bias=epsc[:, 0:1])
            ACT(sm[:, 4:5], sm[:, 3:4], AF.Exp, [sm], [sm], scale=-0.5)
            TSo("dve", u[:], u[:], sm[:, 0:1], ALU.subtract, [u, sm], [u], s2=sm[:, 4:5], op1=ALU.mult)
            TTo("dve", u[:], u[:], lg[:], ALU.mult, [u, lg], [u])
            TTo("dve", X[g][:], u[:], lb[:], ALU.add, [u, lb], [X[g]])

        tmpU = Qb[3]
        epsc = sb([128, 1], name="epsc")
        MSET("dve", epsc[:], float(EPS), [epsc])

        Km = sb([128, 512], BF16, name="Km")
        Kms = [sb([128, 512], BF16, name="Kms%d" % i) for i in range(2)]
        Qms = sb([128, 16, 128], BF16, name="Qms")
        scm = sb([128, 512], BF16, name="scm")
        Sbf = sb([128, 16, 64], BF16, name="Sbf")

        cur_kind = ["p"]
        arena = {}

        def A(name, shape, dt=F32):
            if name not in arena:
                arena[name] = sb(shape, dt, name=name)
            return arena[name]

        arena["qT"] = Pb[0]
        arena["kT"] = Pb[1]
        arena["laT"] = Pb[2]
        arena["gaT"] = Pb[3]
        FMf = FM[:].rearrange("p a b -> p (a b)")
        arena["cacc"] = View(FM, FMf[:, GM * 896:GM * 1024])
        arena["cacc"].b = Buf("cacc")
        arena["ys"] = View(Qb[2], Qb[2][:, 512:1024])
        arena["ce"] = View(Qb[3], Qb[3][:, 512:1024].rearrange("p (h n) -> p h n", h=8))
        arena["sact"] = Pb[0]
        arena["tact"] = Pb[1]
        for g_ in range(GM):
            arena["rq%d" % g_] = View(FM, FMf[:, g_ * 256:(g_ + 1) * 256])
            arena["rk%d" % g_] = View(FM, FMf[:, GM * 256 + g_ * 256:GM * 256 + (g_ + 1) * 256])
            arena["srg%d" % g_] = View(FM, FMf[:, GM * 512 + g_ * 256:GM * 512 + (g_ + 1) * 256])
            arena["vr%d" % g_] = View(FM, FMf[:, GM * 768 + g_ * 128:GM * 768 + (g_ + 1) * 128].bitcast(BF16))
            arena["szs%d" % g_] = View(Qb[g_], Qb[g_][:, 0:512])
            arena["h32_%d" % g_] = View(Qb[g_], Qb[g_][:].rearrange("p (a b) -> p a b", a=8))
        arena["snew"] = snew
        arena["sns"] = snew
        arena["dtr"] = View(snew, snew[:].rearrange("p a b -> p (a b)").rearrange("p (h j) -> p h j", h=8))
        arena["rqtm"] = A("qtm", [128, 4, 128], BF16)
        arena["sel"] = View(Qms, Qms[0:16, :, :])
        arena["c32"] = View(tmpA, tmpA[0:16, 0:128])
        arena["me"] = View(tmpB, tmpB[:, 0:64])
        arena["rt"] = View(tmpB, tmpB[:, 64:128])
        arena["lg"] = View(tmpB, tmpB[:, 128:148])
        cvoA = View(Qb[0], Qb[0][:, 512:1024])
        cvoB = View(Qb[1], Qb[1][:, 512:768])

        for gi, (kind, tiles) in enumerate(groups):
            G = len(tiles)
            NT = G * 128
            nseg = 1 if kind == "p" else 16
            SL = 128 // nseg
            if kind != cur_kind[0]:
                DMA("sp", cst[:], consts_s, [], [cst])
                cur_kind[0] = kind
            for g in range(G):
                DMA("sp", X[g][:], x_all[tiles[g]], [], [X[g]])
                DMA("sp", trig_t[:, g, :], trig[tiles[g]], [], [trig_t])
            last_prompt = (kind == "p" and tiles[-1] == 15)

            KST = int(os.environ.get("KSTAGE", "9"))
            for l in range(n_layers):
                if KST == 0:
                    continue
                if kind == "s" and os.environ.get("KSG") == "0":
                    continue
                TRI = cst[:, C_TRI:C_TRI + 128]
                DMA("sp", rows_t[l][:], rows[l].partition_broadcast(128), [], [rows_t[l]])
                ACT(a_neg[l][:], rows_t[l][:, 1032:1040], AF.Exp, [rows_t[l]], [a_neg[l]])
                TSo("dve", a_neg[l][:], a_neg[l][:], -1.0, ALU.mult, [a_neg[l]], [a_neg[l]])
                make_HT(kind, l, G, 1, 0)
                KSG = int(os.environ.get("KSG", "9")) if kind == "s" else 9
                if KSG <= 1:
                    continue
                mergedT = A("mergedT", [128, 8, NTM], BF16)
                mTb = [Buf("mT%d" % g) for g in range(GM)]

                QD = bass.AP(Qms[:].tensor, 0, [[Qms[:].ap[0][0], 128], [136, 16], [1, 8]])

                def U_mm(pu, items):
                    n = len(items)
                    for i, (lk, lkT, va, vT) in enumerate(items):
                        if kind == "p":
                            MM(pu[0][:, 0:64], lk, va, i == 0, i == n - 1, [lkT, vT], [pu[0]])
                        else:
                            for hf in range(2):
                                TTo("dve", Kms[hf][:].rearrange("p (s v) -> p s v", s=8), va.unsqueeze(1).to_broadcast([128, 8, 64]),
                                    cst[:, C_SEGIND + hf * 8:C_SEGIND + hf * 8 + 8].unsqueeze(2).to_broadcast([128, 8, 64]), ALU.mult,
                                    [vT, cst], [Kms[hf]])
                                MM(pu[hf][:, 0:512], lk, Kms[hf][:], i == 0, i == n - 1, [lkT, Kms[hf]], [pu[hf]])

                def emit_merged(g, mo, c0, nchunk, r):
                    pb = PSA()
                    for q in range(nchunk):
                        TR(pb[:, q * 128:(q + 1) * 128], mo[:, q * 128:(q + 1) * 128], ID, r + [cst], [pb])
                    ACT(mergedT[:, c0:c0 + nchunk, g * 128:(g + 1) * 128],
                        pb[:, 0:nchunk * 128].rearrange("p (a b) -> p a b", a=nchunk), AF.Copy, [pb], [mTb[g]])
                    PSF(pb)

                qT = A("qT", [128, NTM])
                kT = A("kT", [128, NTM])
                gaT = A("gaT", [16, NTM])
                vg = [A("vg%d" % g, [128, 256], BF16) for g in range(GM)]
                sgr = [A("sgr%d" % g, [128, 256]) for g in range(GM)]
                laT = A("laT", [128, NTM])
                sl = w_in_chunk(l, 0, 512)
                fm_proj(sl, 0, 128, NT, G, lambda pb: ACT(qT[:, 0:NT], pb[:, 0:NT], AF.Copy, [pb], [qT], scale=float(32 ** -0.5)))
                fm_proj(sl, 128, 128, NT, G, lambda pb: CP("dve", kT[:, 0:NT], pb[:, 0:NT], [pb], [kT]))
                for g in range(G):
                    tm_proj(sl, 256, 256, g, lambda pb, g=g: ACT(vg[g][:], pb[:, 0:256], AF.Copy, [pb], [vg[g]]))
                sl = w_in_chunk(l, 512, 784)
                fm_proj(sl, 0, 16, NT, G, lambda pb: CP("dve", gaT[0:16, 0:NT], pb[0:16, 0:NT], [pb], [gaT]))
                for g in range(G):
                    tm_proj(sl, 16, 256, g, lambda pb, g=g: ACT(sgr[g][:], pb[:, 0:256], AF.Silu, [pb], [sgr[g]]))
                pb = PS()
                MM(pb[:, 0:NT], wg_t[:, l, :], gaT[0:16, 0:NT], True, True, [wg_t, gaT], [pb])
                ACT(laT[:, 0:NT], pb[:, 0:NT], AF.Exp, [pb, nbg_t], [laT], bias=nbg_t[:, l:l + 1], scale=-1.0)
                TSo("dve", laT[:, 0:NT], laT[:, 0:NT], 1.0, ALU.add, [laT], [laT])
                ACT(laT[:, 0:NT], laT[:, 0:NT], AF.Ln, [laT], [laT])
                def gla_chain():
                    for g in range(G):
                        tk = slice(g * 128, (g + 1) * 128)
                        la_tm = A("la_tm", [128, 128])
                        eg = A("eg", [128, 128])
                        eng_ = A("eng", [128, 128])
                        alast = A("alast", [128, 16])
                        qtm = A("qtm", [128, 4, 128], BF16)
                        ktl = A("ktl", [128, 128], BF16)
                        pb = PSA()
                        TR(pb[:, 0:128], laT[:, tk], ID, [laT, cst], [pb])
                        ACT(la_tm[:], pb[:, 0:128], AF.Copy, [pb], [la_tm], scale=-1.0 / 16.0)
                        PSF(pb)
                        yield
                        pg = PSA()
                        MM(pg[:, 0:128], la_tm[:], TRI, True, True, [la_tm, cst], [pg])
                        ACT(eg[:], pg[:, 0:128], AF.Exp, [pg], [eg])
                        ACT(eng_[:], pg[:, 0:128], AF.Exp, [pg], [eng_], scale=-1.0)
                        ACT(alast[:, 0:nseg], pg[:, SL - 1:128:SL], AF.Exp, [pg], [alast])
                        PSF(pg)
                        yield
                        for h in range(4):
                            STT(qtm[:, h, :], qT[:, tk], cst[:, C_HM4 + h:C_HM4 + h + 1], eg[:], ALU.mult, ALU.mult,
                                [qT, cst, eg], [qtm])
                        TTo("pool", ktl[:], kT[:, tk], eng_[:], ALU.mult, [kT, eng_], [ktl])
                        yield
                        pk = PSA()
                        pkb = pk[:].bitcast(BF16)
                        TR(pkb[:, 0:128], ktl[:], identb[:], [ktl, identb], [pk])
                        if g == 0 and l == 0 and gi == 0:
                            pass
                        MSET("pool", Km[:], 0.0, [Km])
                        for h in range(4):
                            ACT(Km[:, h * 128 + h * 32:h * 128 + h * 32 + 32], pkb[:, h * 32:(h + 1) * 32], AF.Copy, [pk], [Km])
                        PSF(pk)
                        yield
                        psc = PSA()
                        for h in range(4):
                            MM(psc[:, h * 128:(h + 1) * 128], ktl[:], qtm[:, h, :], True, True, [ktl, qtm], [psc])
                        TTo("dve", scm[:].rearrange("p (h i) -> p h i", h=4), psc[:].rearrange("p (h i) -> p h i", h=4),
                            TRI.unsqueeze(1).to_broadcast([128, 4, 128]), ALU.mult, [psc, cst], [scm])
                        PSF(psc)
                        yield
                        po = PSA()
                        if kind == "p":
                            CP("pool", Sbf[:, 0, :], Sg[l][:], [Sg[l]], [Sbf])
                        else:
                            DMA("sp", S0[:], st_gla[l].rearrange("s h k v -> (h k) s v"), [], [S0])
                            CP("dve", Sbf[:], S0[:], [S0], [Sbf])
                        for h in range(4):
                            MM(po[:, h * 64:(h + 1) * 64], scm[:, h * 128:(h + 1) * 128], vg[g][:, h * 64:(h + 1) * 64], True, False,
                               [scm, vg[g]], [po])
                            if kind == "p":
                                MM(po[:, h * 64:(h + 1) * 64], qtm[:, h, :], Sbf[:, 0, :], False, True, [qtm, Sbf], [po])
                            else:
                                CP("dve", QD, qtm[:, h, :].rearrange("p (s t) -> p s t", s=16), [qtm], [Qms])
                                for s in range(16):
                                    MM(po[:, h * 64:(h + 1) * 64], Qms[:, s, :], Sbf[:, s, :], False, s == 15, [Qms, Sbf], [po])
                        yield
                        og = A("og", [128, 256])
                        ACT(og[:], po[:, 0:256], AF.Copy, [po], [og])
                        PSF(po)
                        pu = [PSA(), PSA() if nseg > 8 else None]
                        U_mm(pu, [(Km[:, h * 128:(h + 1) * 128], Km, vg[g][:, h * 64:(h + 1) * 64], vg[g]) for h in range(4)])
                        if kind == "p":
                            yield
                            TTo("dve", tmpB[:, 256:320], pu[0][:, 0:64], Sg[l][:], ALU.add, [pu[0], Sg[l]], [tmpB])
                            TSo("dve", Sg[l][:], tmpB[:, 256:320], alast[:, 0:1], ALU.mult, [tmpB, alast], [Sg[l]])
                            if last_prompt and g == G - 1:
                                DMA("sp", o_gla_p[l], Sg[l][:], [Sg[l]], [])
                                outs.append(Sg[l])
                        else:
                            sn = A("snew", [128, 16, 64])
                            for hf in range(2):
                                TTo("dve", tmpA[:].rearrange("p (s v) -> p s v", s=8), pu[hf][:].rearrange("p (s v) -> p s v", s=8),
                                    S0[:, hf * 8:(hf + 1) * 8, :], ALU.add, [pu[hf], S0], [tmpA])
                                TTo("dve", sn[:, hf * 8:(hf + 1) * 8, :], tmpA[:].rearrange("p (s v) -> p s v", s=8),
                                    alast[:, hf * 8:(hf + 1) * 8].unsqueeze(2).to_broadcast([128, 8, 64]), ALU.mult,
                                    [tmpA, alast], [sn])
                            DMA("sp", o_gla_s[l], sn[:], [sn], [])
                            outs.append(sn)
                        PSF(pu[0])
                        if pu[1] is not None:
                            PSF(pu[1])
                        yield
                        TTo("pool", tmpB[:, 0:256], og[:], og[:], ALU.mult, [og], [tmpB])
                        S.op("dve", lambda e: e.tensor_reduce(sm[:, 8:12], tmpB[:, 0:256].rearrange("p (h v) -> p h v", h=4), AX.X, ALU.add),
                             [tmpB], [sm])
                        yield
                        ACT(sm[:, 12:16], sm[:, 8:12], AF.Ln, [sm, epsc], [sm], bias=epsc[:, 0:1], scale=1.0 / 64.0)
                        ACT(sm[:, 16:20], sm[:, 12:16], AF.Exp, [sm], [sm], scale=-0.5)
                        TTo("pool", tmpB[:, 0:256], sgr[g][:], rows_t[l][:, 0:256], ALU.mult, [sgr[g], rows_t[l]], [tmpB])
                        TTo("dve", og[:].rearrange("p (h v) -> p h v", h=4), og[:].rearrange("p (h v) -> p h v", h=4),
                            sm[:, 16:20].unsqueeze(2).to_broadcast([128, 4, 64]), ALU.mult, [og, sm], [og])
                        TTo("dve", og[:], og[:], tmpB[:, 0:256], ALU.mult, [og, tmpB], [og])
                        yield
                        emit_merged(g, og, 0, 2, [og])
                        yield


                if KSG <= 2:
                    continue
                rq = [A("rq%d" % g, [128, 256]) for g in range(GM)]
                rk = [A("rk%d" % g, [128, 256]) for g in range(GM)]
                vr = [A("vr%d" % g, [128, 256], BF16) for g in range(GM)]
                srg = [A("srg%d" % g, [128, 256]) for g in range(GM)]
                sl = w_in_chunk(l, 784, 1296)
                for g in range(G):
                    tm_proj(sl, 0, 256, g, lambda pb, g=g: ACT(rq[g][:], pb[:, 0:256], AF.Copy, [pb], [rq[g]]))
                    tm_proj(sl, 256, 256, g, lambda pb, g=g: ACT(rk[g][:], pb[:, 0:256], AF.Copy, [pb], [rk[g]], scale=0.125))
                sl = w_in_chunk(l, 1296, 1808)
                for g in range(G):
                    tm_proj(sl, 0, 256, g, lambda pb, g=g: ACT(vr[g][:], pb[:, 0:256], AF.Copy, [pb], [vr[g]]))
                    tm_proj(sl, 256, 256, g, lambda pb, g=g: ACT(srg[g][:], pb[:, 0:256], AF.Silu, [pb], [srg[g]]))
                def ret_chain():
                    KmR = Kms[0] if kind == "p" else Km
                    scmR = Kms[1] if kind == "p" else scm
                    for g in range(G):
                        cos2 = trig_t[:, g, 0:64].unsqueeze(1).to_broadcast([128, 4, 64])
                        nsin = trig_t[:, g, 64:96].unsqueeze(1).to_broadcast([128, 4, 32])
                        psin = trig_t[:, g, 96:128].unsqueeze(1).to_broadcast([128, 4, 32])
                        ropes = []
                        for nm, src, eng in (("q", rq[g], "dve"), ("k", rk[g], "pool")):
                            t1 = A("rp1" + nm, [128, 256])
                            t2 = A("rp2" + nm, [128, 256])
                            s3 = src[:].rearrange("p (h k) -> p h k", h=4)
                            TTo(eng, t1[:].rearrange("p (h k) -> p h k", h=4), s3, cos2, ALU.mult, [src, trig_t], [t1])
                            TTo(eng, t2[:].rearrange("p (h k) -> p h k", h=4)[:, :, 0:32], s3[:, :, 32:64], nsin, ALU.mult, [src, trig_t], [t2])
                            TTo(eng, t2[:].rearrange("p (h k) -> p h k", h=4)[:, :, 32:64], s3[:, :, 0:32], psin, ALU.mult, [src, trig_t], [t2])
                            TTo(eng, t1[:], t1[:], t2[:], ALU.add, [t1, t2], [t1])
                            ropes.append(t1)
                        qr, kr = ropes
                        yield
                        if kind == "p":
                            qtm = View(Qms, Qms[:, 0:4, :])
                        else:
                            qtm = A("rqtm", [128, 4, 128], BF16)
                        qhm = A("rqhm", [128, 4, 128], BF16)
                        krT = A("krT", [128, 2, 128], BF16)
                        pq = PSA()
                        for u in range(2):
                            TR(pq[:, u * 128:(u + 1) * 128], qr[:, u * 128:(u + 1) * 128], ID, [qr, cst], [pq])
                            TR(pq[:, 256 + u * 128:256 + (u + 1) * 128], kr[:, u * 128:(u + 1) * 128], ID, [kr, cst], [pq])
                        ACT(tmpA[:, 256:512], pq[:, 0:256], AF.Copy, [pq], [tmpA])
                        ACT(krT[:].rearrange("p a b -> p (a b)"), pq[:, 256:512], AF.Copy, [pq], [krT])
                        PSF(pq)
                        yield
                        for u in range(2):
                            for hl in range(2):
                                h = 2 * u + hl
                                TSo("dve", qtm[:, h, :], tmpA[:, 256 + u * 128:256 + (u + 1) * 128], cst[:, C_HM2 + hl:C_HM2 + hl + 1], ALU.mult,
                                    [tmpA, cst], [qtm])
                                STT(qhm[:, h, :], tmpA[:, 256 + u * 128:256 + (u + 1) * 128], cst[:, C_HM2 + hl:C_HM2 + hl + 1],
                                    cst[:, C_GQ + u * 128:C_GQ + (u + 1) * 128], ALU.mult, ALU.mult, [tmpA, cst], [qhm])
                        yield
                        MSET("pool", KmR[:], 0.0, [KmR])
                        for hl in range(2):
                            TTo("dve", KmR[:].rearrange("p (u x) -> p u x", u=2)[:, :, hl * 128 + hl * 64:hl * 128 + hl * 64 + 64],
                                kr[:].rearrange("p (u x) -> p u x", u=2)[:, :, hl * 64:(hl + 1) * 64],
                                cst[:, C_GK + hl:C_GK + hl + 3:2].unsqueeze(2).to_broadcast([128, 2, 64]), ALU.mult,
                                [kr, cst], [KmR])
                        yield
                        psc = PSA()
                        for h in range(4):
                            MM(psc[:, h * 128:(h + 1) * 128], krT[:, h // 2, :], qtm[:, h, :], True, True, [krT, qtm], [psc])
                        TTo("dve", scmR[:], psc[:], cst[:, C_DM:C_DM + 512], ALU.mult, [psc, cst], [scmR])
                        PSF(psc)
                        yield
                        po = PSA()
                        snr = snew
                        for u in range(2):
                            if kind == "p":
                                CP("pool", Sbf[:, 1 + u, :], Sr[l][u][:], [Sr[l][u]], [Sbf])
                            else:
                                DMA("sp", S0[:], st_ret[l][:, 2 * u:2 * u + 2].rearrange("s h k v -> (h k) s v"), [], [S0])
                                CP("dve", Sbf[:], S0[:], [S0], [Sbf])
                            for hl in range(2):
                                h = 2 * u + hl
                                MM(po[:, h * 64:(h + 1) * 64], scmR[:, h * 128:(h + 1) * 128], vr[g][:, h * 64:(h + 1) * 64], True, False,
                                   [scmR, vr[g]], [po])
                                if kind == "p":
                                    MM(po[:, h * 64:(h + 1) * 64], qhm[:, h, :], Sbf[:, 1 + u, :], False, True, [qhm, Sbf], [po])
                                else:
                                    CP("dve", QD, qhm[:, h, :].rearrange("p (s t) -> p s t", s=16), [qhm], [Qms])
                                    for s in range(16):
                                        MM(po[:, h * 64:(h + 1) * 64], Qms[:, s, :], Sbf[:, s, :], False, s == 15, [Qms, Sbf], [po])
                            yield
                            pu = [PSA(), PSA() if nseg > 8 else None]
                            U_mm(pu, [(KmR[:, (2 * u + hl) * 128:(2 * u + hl + 1) * 128], KmR,
                                       vr[g][:, (2 * u + hl) * 64:(2 * u + hl + 1) * 64], vr[g]) for hl in range(2)])
                            if kind == "p":
                                STT(Sr[l][u][:], Sr[l][u][:], cst[:, C_AL + u:C_AL + u + 1], pu[0][:, 0:64], ALU.mult, ALU.add,
                                    [Sr[l][u], cst, pu[0]], [Sr[l][u]])
                                if last_prompt and g == G - 1:
                                    DMA("sp", o_ret_p[l, u], Sr[l][u][:], [Sr[l][u]], [])
                                    outs.append(Sr[l][u])
                            else:
                                for hf in range(2):
                                    STT(snr[:, hf * 8:(hf + 1) * 8, :], S0[:, hf * 8:(hf + 1) * 8, :], cst[:, C_AL + u:C_AL + u + 1],
                                        pu[hf][:].rearrange("p (s v) -> p s v", s=8), ALU.mult, ALU.add, [S0, cst, pu[hf]], [snr])
                                DMA("sp", o_ret_s[l, u], snr[:], [snr], [])
                                outs.append(snr)
                            PSF(pu[0])
                            if pu[1] is not None:
                                PSF(pu[1])
                        yield
                        orr = A("orr", [128, 256])
                        ACT(orr[:], po[:, 0:256], AF.Copy, [po], [orr])
                        PSF(po)
                        yield
                        o3 = orr[:].rearrange("p (h v) -> p h v", h=4)
                        S.op("dve", lambda e, o3=o3: e.tensor_reduce(sm[:, 20:24], o3, AX.X, ALU.add), [orr], [sm])
                        TSo("dve", sm[:, 20:24], sm[:, 20:24], 1.0 / 64.0, ALU.mult, [sm], [sm])
                        TTo("dve", o3, o3, sm[:, 20:24].unsqueeze(2).to_broadcast([128, 4, 64]), ALU.subtract, [orr, sm], [orr])
                        TTo("pool", tmpA[:, 0:256], orr[:], orr[:], ALU.mult, [orr], [tmpA])
                        S.op("dve", lambda e: e.tensor_reduce(sm[:, 24:28], tmpA[:, 0:256].rearrange("p (h v) -> p h v", h=4), AX.X, ALU.add),
                             [tmpA], [sm])
                        yield
                        ACT(sm[:, 28:32], sm[:, 24:28], AF.Ln, [sm, epsc], [sm], bias=epsc[:, 0:1], scale=1.0 / 64.0)
                        ACT(sm[:, 32:36], sm[:, 28:32], AF.Exp, [sm], [sm], scale=-0.5)
                        TTo("pool", tmpA[:, 0:256], srg[g][:], rows_t[l][:, 256:512], ALU.mult, [srg[g], rows_t[l]], [tmpA])
                        TTo("dve", o3, o3, sm[:, 32:36].unsqueeze(2).to_broadcast([128, 4, 64]), ALU.mult, [orr, sm], [orr])
                        TTo("dve", orr[:], orr[:], tmpA[:, 0:256], ALU.mult, [orr, tmpA], [orr])
                        yield
                        emit_merged(g, orr, 2, 2, [orr])
                        yield


                TT_ = NT // nseg
                szs = [A("szs%d" % g, [128, 512]) for g in range(GM)]
                sx = A("sx", [128, 6, NTM + 48])
                xbc = A("xbcT", [128, 6, NTM], BF16)
                dtx = A("dtx", [128, GM, 8])

                def sxv(c):
                    return sx[:, c, 0:nseg * (TT_ + 3)].rearrange("p (s t) -> p s t", s=nseg)

                if kind == "p":
                    CP("dve", sx[:, :, 0:3], chist[l][:], [chist[l]], [sx])
                else:
                    for c in range(6):
                        DMA("sp", sxv(c)[:, :, 0:3], st_conv[l][:, c], [], [sx])
                sl = w_in_chunk(l, 1808, 2320)
                for g in range(G):
                    tm_proj(sl, 0, 512, g, lambda pb, g=g: ACT(szs[g][:], pb[:, 0:512], AF.Silu, [pb], [szs[g]]))
                need_conv_out = last_prompt or kind == "s"
                for (c0, c1, chs) in ((2320, 2832, [0, 1, 2, 3]), (2832, 3096, [4, 5])):
                    sl = w_in_chunk(l, c0, c1)
                    for qi, c in enumerate(chs):
                        fm_proj(sl, qi * 128, 128, NT, G,
                                lambda pb, c=c: CP("dve", sxv(c)[:, :, 3:3 + TT_], pb[:, 0:NT].rearrange("p (s t) -> p s t", s=nseg),
                                                   [pb], [sx]))
                    if c0 == 2832:
                        for g in range(G):
                            tm_proj(sl, 256, 8, g, lambda pb, g=g: TTo("dve", dtx[:, g, :], pb[:, 0:8], rows_t[l][:, 1024:1032], ALU.add,
                                                                        [pb, rows_t[l]], [dtx]))
                    if need_conv_out:
                        g = G - 1
                        if c0 == 2320:
                            tm_proj(sl, 0, 512, g, lambda pb: ACT(cvoA[:, 0:512], pb[:, 0:512], AF.Copy, [pb], [cvoA]))
                        else:
                            tm_proj(sl, 0, 256, g, lambda pb: ACT(cvoB[:, 0:256], pb[:, 0:256], AF.Copy, [pb], [cvoB]))
                if need_conv_out:
                    if kind == "p":
                        DMA("sp", o_conv_p[l][:, 0:512], cvoA[125:128, :], [cvoA], [])
                        DMA("sp", o_conv_p[l][:, 512:768], cvoB[125:128, :], [cvoB], [])
                    else:
                        for j in range(3):
                            dv = o_conv_s[l].rearrange("(s j) c -> j s c", j=3)[j]
                            DMA("sp", dv[:, 0:512], cvoA[5 + j:128:8, :], [cvoA], [])
                            DMA("sp", dv[:, 512:768], cvoB[5 + j:128:8, :], [cvoB], [])
                if kind == "p":
                    CP("dve", chist[l][:], sx[:, :, NT:NT + 3], [sx], [chist[l]])
                def ssd_chain():
                    dt = A("dt", [128, GM, 8])
                    dta = A("dta", [128, GM, 8])
                    d1 = A("d1", [128, GM, 8])
                    ACT(d1[:, 0:G, :], dtx[:, 0:G, :], AF.Abs, [dtx], [d1])
                    ACT(d1[:, 0:G, :], d1[:, 0:G, :], AF.Exp, [d1], [d1], scale=-1.0)
                    TSo("dve", d1[:, 0:G, :], d1[:, 0:G, :], 1.0, ALU.add, [d1], [d1])
                    ACT(d1[:, 0:G, :], d1[:, 0:G, :], AF.Ln, [d1], [d1])
                    TSo("dve", dt[:, 0:G, :], dtx[:, 0:G, :], 0.0, ALU.max, [dtx], [dt])
                    TTo("dve", dt[:, 0:G, :], dt[:, 0:G, :], d1[:, 0:G, :], ALU.add, [dt, d1], [dt])
                    TTo("dve", dta[:, 0:G, :], dt[:, 0:G, :], a_neg[l][:].unsqueeze(1).to_broadcast([128, G, 8]), ALU.mult,
                        [dt, a_neg[l]], [dta])
                    yield
                    cacc = A("cacc", [128, NTM])
                    for c in range(6):
                        cv = cacc[:, 0:NT].rearrange("p (s t) -> p s t", s=nseg)
                        TSo("dve", cv, sxv(c)[:, :, 0:TT_], cw_t[:, l, c, 0:1], ALU.mult, [sx, cw_t, cb_t], [cacc],
                            s2=cb_t[:, l, c:c + 1], op1=ALU.add)
                        for i in range(1, 4):
                            STT(cv, sxv(c)[:, :, i:i + TT_], cw_t[:, l, c, i:i + 1], cv, ALU.mult, ALU.add, [sx, cw_t, cacc], [cacc])
                        ACT(xbc[:, c, 0:NT], cacc[:, 0:NT], AF.Silu, [cacc], [xbc])
                        yield
                    for g in range(G):
                        tk = slice(g * 128, (g + 1) * 128)
                        xtm = A("xtm", [128, 768], BF16)
                        for half in range(2):
                            pt = PSA()
                            ptb = pt[:].bitcast(BF16)
                            for q in range(3):
                                c = half * 3 + q
                                TR(ptb[:, q * 128:(q + 1) * 128], xbc[:, c, tk], identb[:], [xbc, identb], [pt])
                            if half == 0:
                                ACT(xtm[:, 0:384], ptb[:, 0:384], AF.Copy, [pt], [xtm])
                            else:
                                ACT(xtm[:, 384:768], ptb[:, 0:384], AF.Copy, [pt], [xtm])
                            PSF(pt)
                            yield
                        vs = A("vs", [128, 512], BF16)
                        TTo("pool", vs[:].rearrange("p (h v) -> p h v", h=8), xtm[:, 0:512].rearrange("p (h v) -> p h v", h=8),
                            dt[:, g, :].unsqueeze(2).to_broadcast([128, 8, 64]), ALU.mult, [xtm, dt], [vs])
                        yield
                        pg = PSA()
                        MM(pg[:, 0:8], TRI, dta[:, g, :], True, True, [cst, dta], [pg])
                        MM(pg[:, 8:16], cst[:, C_SEGALL:C_SEGALL + 128], dta[:, g, :], True, True, [cst, dta], [pg])
                        negg = A("negg", [128, 8])
                        ei = A("ei", [128, 8])
                        fj = A("fj", [128, 8])
                        ACT(negg[:], pg[:, 0:8], AF.Copy, [pg], [negg], scale=-1.0)
                        ACT(ei[:], pg[:, 0:8], AF.Exp, [pg], [ei])
                        TTo("dve", fj[:], pg[:, 8:16], negg[:], ALU.add, [pg, negg], [fj])
                        PSF(pg)
                        yield
                        ACT(fj[:], fj[:], AF.Exp, [fj], [fj])
                        dtr = A("dtr", [128, 8, 128])
                        CP("pool", dtr[:], dta[:, g, :].unsqueeze(2).to_broadcast([128, 8, 128]), [dta], [dtr])
                        als = A("als", [128, 4, 16])
                        yield
                        pa = PSA()
                        for h in range(8):
                            MM(pa[:, h * 16:h * 16 + nseg], dtr[:, h, :], cst[:, C_SEGIND:C_SEGIND + nseg], True, True,
                               [dtr, cst], [pa])
                        for hl in range(2):
                            ACT(als[hl * 64:(hl + 1) * 64, :, 0:nseg],
                                pa[hl * 64:(hl + 1) * 64, 0:128].rearrange("p (u x) -> p u x", u=4)[:, :, hl * 16:hl * 16 + nseg],
                                AF.Exp, [pa], [als])
                        PSF(pa)
                        yield
                        Lm = A("Lm", [128, 8, 128])
                        for hh in range(2):
                            pl = PSA()
                            for q in range(4):
                                h = hh * 4 + q
                                MM(pl[:, q * 128:(q + 1) * 128], dtr[:, h, :], TRI, True, False, [dtr, cst], [pl])
                                MM(pl[:, q * 128:(q + 1) * 128], ID, cst[:, C_NEG:C_NEG + 128], False, True, [cst], [pl])
                            for q in range(4):
                                h = hh * 4 + q
                                ACT(Lm[:, h, :], pl[:, q * 128:(q + 1) * 128], AF.Exp, [pl, negg], [Lm], bias=negg[:, h:h + 1])
                            PSF(pl)
                            yield
                        ctm = A("ctm", [128, 2, 128], BF16)
                        for gg in range(2):
                            TSo("pool", ctm[:, gg, :], xbc[:, 5, tk], cst[:, C_HM2 + gg:C_HM2 + gg + 1], ALU.mult, [xbc, cst], [ctm])
                        pcb = PSA()
                        for gg in range(2):
                            MM(pcb[:, gg * 128:(gg + 1) * 128], xbc[:, 4, tk], ctm[:, gg, :], True, True, [xbc, ctm], [pcb])
                        scm8 = A("scm8", [128, 8, 128], BF16)
                        for gg in range(2):
                            TTo("dve", scm8[:, gg * 4:(gg + 1) * 4, :], Lm[:, gg * 4:(gg + 1) * 4, :],
                                pcb[:, gg * 128:(gg + 1) * 128].unsqueeze(1).to_broadcast([128, 4, 128]), ALU.mult, [Lm, pcb], [scm8])
                        PSF(pcb)
                        yield
                        ce = A("ce", [128, 8, 64])
                        for gg in range(2):
                            TTo("pool", ce[:, gg * 4:(gg + 1) * 4, :],
                                xtm[:, 640 + gg * 64:640 + (gg + 1) * 64].unsqueeze(1).to_broadcast([128, 4, 64]),
                                ei[:, gg * 4:(gg + 1) * 4].unsqueeze(2).to_broadcast([128, 4, 64]), ALU.mult, [xtm, ei], [ce])
                        xm = A("xm", [128, 8, 128], BF16)
                        pce = PSA()
                        for u in range(4):
                            TR(pce[:, u * 128:(u + 1) * 128], ce[:, 2 * u:2 * u + 2, :].rearrange("p a b -> p (a b)"), ID, [ce, cst], [pce])
                        ACT(tmpC[:], pce[:], AF.Copy, [pce], [tmpC])
                        PSF(pce)
                        yield
                        for u in range(4):
                            for hl in range(2):
                                TSo("dve", xm[:, 2 * u + hl, :], tmpC[:, u * 128:(u + 1) * 128], cst[:, C_HM2 + hl:C_HM2 + hl + 1], ALU.mult,
                                    [tmpC, cst], [xm])
                        yield
                        Bm = A("Bm", [128, 8, 128], BF16)
                        MSET("pool", Bm[:], 0.0, [Bm])
                        for h in range(8):
                            hl = h % 2
                            gg = h // 4
                            TSo("pool" if h % 2 else "dve", Bm[:, h, hl * 64:(hl + 1) * 64], xtm[:, 512 + gg * 64:512 + (gg + 1) * 64], fj[:, h:h + 1], ALU.mult,
                                [xtm, fj], [Bm])
                        yield
                        po = PSA()
                        pu_ssd = [PSA(), PSA() if nseg > 8 else None]
                        sns = A("sns", [128, 16, 64])
                        for u in range(4):
                            yield
                            if kind == "p":
                                CP("pool", Sbf[:, 3 + u, :], Ss[l][u][:], [Ss[l][u]], [Sbf])
                            else:
                                DMA("sp", S0[:], st_ssd[l][:, 2 * u:2 * u + 2].rearrange("s h k v -> (h k) s v"), [], [S0])
                                CP("dve", Sbf[:], S0[:], [S0], [Sbf])
                            for hl in range(2):
                                h = 2 * u + hl
                                MM(po[:, h * 64:(h + 1) * 64], scm8[:, h, :], vs[:, h * 64:(h + 1) * 64], True, False, [scm8, vs], [po])
                                if kind == "p":
                                    MM(po[:, h * 64:(h + 1) * 64], xm[:, h, :], Sbf[:, 3 + u, :], False, True, [xm, Sbf], [po])
                                else:
                                    CP("dve", QD, xm[:, h, :].rearrange("p (s t) -> p s t", s=16), [xm], [Qms])
                                    for s in range(16):
                                        MM(po[:, h * 64:(h + 1) * 64], Qms[:, s, :], Sbf[:, s, :], False, s == 15, [Qms, Sbf], [po])
                            pu = pu_ssd
                            U_mm(pu, [(Bm[:, 2 * u + hl, :], Bm, vs[:, (2 * u + hl) * 64:(2 * u + hl + 1) * 64], vs) for hl in range(2)])
                            if kind == "p":
                                STT(Ss[l][u][:], Ss[l][u][:], als[:, u, 0:1], pu[0][:, 0:64], ALU.mult, ALU.add,
                                    [Ss[l][u], als, pu[0]], [Ss[l][u]])
                                if last_prompt and g == G - 1:
                                    DMA("sp", o_ssd_p[l, u], Ss[l][u][:], [Ss[l][u]], [])
                                    outs.append(Ss[l][u])
                            else:
                                for hf in range(2):
                                    TTo("dve", tmpA[:].rearrange("p (s v) -> p s v", s=8), S0[:, hf * 8:(hf + 1) * 8, :],
                                        als[:, u, hf * 8:(hf + 1) * 8].unsqueeze(2).to_broadcast([128, 8, 64]), ALU.mult, [S0, als], [tmpA])
                                    TTo("dve", sns[:, hf * 8:(hf + 1) * 8, :], tmpA[:].rearrange("p (s v) -> p s v", s=8),
                                        pu[hf][:].rearrange("p (s v) -> p s v", s=8), ALU.add, [tmpA, pu[hf]], [sns])
                                DMA("sp", o_ssd_s[l, u], sns[:], [sns], [])
                                outs.append(sns)
                        PSF(pu_ssd[0])
                        if pu_ssd[1] is not None:
                            PSF(pu_ssd[1])
                        yield
                        ys = A("ys", [128, 512])
                        TTo("pool", tmpC[:].rearrange("p (h v) -> p h v", h=8), xtm[:, 0:512].rearrange("p (h v) -> p h v", h=8),
                            rows_t[l][:, 1040:1048].unsqueeze(2).to_broadcast([128, 8, 64]), ALU.mult, [xtm, rows_t[l]], [tmpC])
                        TTo("dve", ys[:], tmpC[:], po[:], ALU.add, [tmpC, po], [ys])
                        PSF(po)
                        yield
                        TTo("pool", ys[:], ys[:], szs[g][:], ALU.mult, [ys, szs[g]], [ys])
                        TTo("pool", tmpC[:], ys[:], ys[:], ALU.mult, [ys], [tmpC])
                        S.op("dve", lambda e: e.tensor_reduce(sm[:, 36:37], tmpC[:], AX.X, ALU.add), [tmpC], [sm])
                        yield
                        ACT(sm[:, 37:38], sm[:, 36:37], AF.Ln, [sm, epsc], [sm], bias=epsc[:, 0:1], scale=1.0 / 512.0)
                        ACT(sm[:, 38:39], sm[:, 37:38], AF.Exp, [sm], [sm], scale=-0.5)
                        STT(ys[:], ys[:], sm[:, 38:39], rows_t[l][:, 512:1024], ALU.mult, ALU.mult, [ys, sm, rows_t[l]], [ys])
                        yield
                        emit_merged(g, ys, 4, 4, [ys])
                        yield

                if KSG <= 3:
                    continue
                if kind == "s":
                    MSET("dve", Qms[:], 0.0, [Qms])
                chains_ = [gla_chain(), ret_chain(), ssd_chain()]
                if ADA_IN_CHAIN and gi == 0 and l == 0:
                    chains_.append(emit_ada(1, PSA, PSF))
                run_chains(chains_, kind == "p")

                if os.environ.get("KDBG") == "1" and kind == "s" and l == 0:
                    dbgt = A("dbgt", [128, 8, 128])
                    CP("dve", dbgt[:], mergedT[:, :, 0:128], mTb[:1], [dbgt])
                    DMA("sp", dbg_out, dbgt[:], [dbgt], [])
                if KSG <= 4:
                    continue
                DMA("sp", lnr[0][:], ln_rows[l, 0].partition_broadcast(128), [], [lnr[0]])
                DMA("sp", lnr[1][:], ln_rows[l, 1].partition_broadcast(128), [], [lnr[1]])
                for hf in range(2):
                    sl = next_slot()
                    for q in range(4):
                        dc = hf * 4 + q
                        pb = PS()
                        for kc in range(8):
                            MM(pb[:, 0:NT], sl[:, kc, q * 128:(q + 1) * 128], mergedT[:, kc, 0:NT], kc == 0, kc == 7,
                               [sl] + mTb[:G], [pb])
                        gate_evac(kind, l, 2, dc, FM[:, dc, 0:NT], pb[:, 0:NT], NT, [pb], [FM])
                for g in range(G):
                    pbs = [PS(), PS()]
                    for dc in range(8):
                        TR(pbs[dc // 4][:, (dc % 4) * 128:(dc % 4 + 1) * 128], FM[:, dc, g * 128:(g + 1) * 128], ID, [FM, cst], [pbs[dc // 4]])
                    layer_norm_tile(g, pbs, lnr[0], lnr[1])

                if stop_after == ("mixer", l):
                    break

                if KSG <= 5:
                    continue
                h32 = [A("h32_%d" % g, [128, 8, 128]) for g in range(GM)]
                make_HT(kind, l, G, 4, 3, h32=h32)
                comb = A("comb", [128, GM, 16])
                combT = View(mergedT, mergedT[0:16, 2:4, :])
                LG = tmpB[:, 0:GM * 20].rearrange("p (g n) -> p g n", g=GM)[:, 0:G, :]
                Rr = tmpB[:, 80:80 + GM * 32].rearrange("p (g n) -> p g n", g=GM)[:, 0:G, :]
                ME = tmpB[:, 208:208 + GM * 64].rearrange("p (g n) -> p g n", g=GM)[:, 0:G, :]
                tB = [tmpB]
                pr = PS()
                for g in range(G):
                    for kc in range(8):
                        MM(pr[:, g * 20:(g + 1) * 20], h32[g][:, kc, :], wrt_t[:, l, kc, :], kc == 0, kc == 7, [h32[g], wrt_t], [pr])
                TTo("dve", LG, pr[:, 0:G * 20].rearrange("p (g n) -> p g n", g=G),
                    brt_t[l][:].unsqueeze(1).to_broadcast([128, G, 20]), ALU.add, [pr, brt_t[l]], tB)

                def RED(out, in_, op):
                    S.op("dve", lambda e: e.tensor_reduce(out, in_, AX.X, op), tB, tB)

                def bc(ap, n):
                    return ap.to_broadcast([128, G, n])

                RED(Rr[:, :, 0], LG[:, :, 0:4], ALU.max)
                TTo("dve", Rr[:, :, 1:5], LG[:, :, 0:4], bc(Rr[:, :, 0:1], 4), ALU.subtract, tB, tB)
                TSo("dve", Rr[:, :, 5:9], Rr[:, :, 1:5], 0.0, ALU.is_ge, tB, tB)
                ACT(Rr[:, :, 9:13], Rr[:, :, 1:5], AF.Exp, tB, tB)
                RED(Rr[:, :, 13], Rr[:, :, 9:13], ALU.add)
                S.op("dve", lambda e, a=Rr[:, :, 14], b=Rr[:, :, 13]: e.reciprocal(a, b), tB, tB)
                TSo("dve", Rr[:, :, 16:20], Rr[:, :, 5:9], 1.0e9, ALU.mult, tB, tB, s2=-1.0e9, op1=ALU.add)
                TTo("dve", ME[:, :, 0:16].rearrange("p g (a e) -> p g a e", a=4), LG[:, :, 4:20].rearrange("p g (a e) -> p g a e", a=4),
                    Rr[:, :, 16:20].unsqueeze(3).to_broadcast([128, G, 4, 4]), ALU.add, tB, tB)
                RED(Rr[:, :, 20], ME[:, :, 0:16], ALU.max)
                TTo("dve", ME[:, :, 16:32], ME[:, :, 0:16], bc(Rr[:, :, 20:21], 16), ALU.is_ge, tB, tB)
                STT(ME[:, :, 32:48], ME[:, :, 16:32], -2.0e9, ME[:, :, 0:16], ALU.mult, ALU.add, tB, tB)
                RED(Rr[:, :, 21], ME[:, :, 32:48], ALU.max)
                TTo("dve", ME[:, :, 48:64], ME[:, :, 32:48], bc(Rr[:, :, 21:22], 16), ALU.is_ge, tB, tB)
                TTo("dve", Rr[:, :, 22:23], Rr[:, :, 21:22], Rr[:, :, 20:21], ALU.subtract, tB, tB)
                ACT(Rr[:, :, 23:24], Rr[:, :, 22:23], AF.Exp, tB, tB)
                TSo("dve", Rr[:, :, 24:25], Rr[:, :, 23:24], 1.0, ALU.add, tB, tB)
                S.op("dve", lambda e, a=Rr[:, :, 25:26], b=Rr[:, :, 24:25]: e.reciprocal(a, b), tB, tB)
                TTo("dve", Rr[:, :, 26:27], Rr[:, :, 23:24], Rr[:, :, 25:26], ALU.mult, tB, tB)
                TTo("dve", Rr[:, :, 25:27], Rr[:, :, 25:27], bc(Rr[:, :, 14:15], 2), ALU.mult, tB, tB)
                TTo("dve", comb[:, 0:G, :], ME[:, :, 16:32], bc(Rr[:, :, 25:26], 16), ALU.mult, tB, [comb])
                TTo("dve", ME[:, :, 0:16], ME[:, :, 48:64], bc(Rr[:, :, 26:27], 16), ALU.mult, tB, tB)
                TTo("dve", comb[:, 0:G, :], comb[:, 0:G, :], ME[:, :, 0:16], ALU.add, [comb, tmpB], [comb])
                pc = PS()
                for g in range(G):
                    TR(pc[0:16, g * 128:(g + 1) * 128], comb[:, g, :], ID, [comb, cst], [pc])
                CP("dve", combT[:, 0, 0:NT], pc[0:16, 0:NT], [pc], [combT])
                c32 = FM[0:16, 0, 0:NT]
                CP("dve", c32, combT[:, 0, 0:NT], [combT], [FM])
                TTo("dve", combT[:, 1, 0:NT], pc[0:16, 0:NT], c32, ALU.subtract, [pc, FM], [combT])
                if os.environ.get("KDBG") == "2" and gi == 0 and l == 0:
                    DMA("sp", dbg_out[:, 0, 0:GM * 16], comb[:].rearrange("p a b -> p (a b)"), [comb], [])
                if KSG <= 6:
                    continue
                sel = A("sel", [16, 16, 128], BF16)
                CP("dve", sel[:], cst[0:16, C_ID:C_ID + 16].unsqueeze(2).to_broadcast([16, 16, 128]), [cst], [sel])
                hidT = View(mergedT, mergedT[:, 0:2, :])
                hidTb = [Buf("hid_fc0"), Buf("hid_fc1")]
                sact = A("sact", [128, NTM])
                tact = A("tact", [128, NTM])
                for ex in range(16):
                    sl13 = next_slot()
                    sl2 = next_slot()
                    w2v = w2view(sl2)
                    pbc = PS()
                    MM(pbc[:, 0:NT], sel[:, ex, :], combT[:, 0, 0:NT], True, False, [sel, combT], [pbc])
                    MM(pbc[:, 0:NT], sel[:, ex, :], combT[:, 1, 0:NT], False, True, [sel, combT], [pbc])
                    for fc in range(2):
                        p1 = PS()
                        p3 = PS()
                        for kc in range(8):
                            MM(p1[:, 0:NT], sl13[:, kc, fc * 128:(fc + 1) * 128], HT[:, kc, 0:NT], kc == 0, kc == 7, [sl13] + HTb[:G], [p1])
                        for kc in range(8):
                            MM(p3[:, 0:NT], sl13[:, kc, 256 + fc * 128:256 + (fc + 1) * 128], HT[:, kc, 0:NT], kc == 0, kc == 7,
                               [sl13] + HTb[:G], [p3])
                        ACT(sact[:, 0:NT], p1[:, 0:NT], AF.Silu, [p1], [sact])
                        TTo("dve", tact[:, 0:NT], sact[:, 0:NT], p3[:, 0:NT], ALU.mult, [sact, p3], [tact])
                        TTo("dve", hidT[:, fc, 0:NT], tact[:, 0:NT], pbc[:, 0:NT], ALU.mult, [tact, pbc], [hidTb[fc]])
                    for dh in range(2):
                        pys = [PS() for _ in range(4)]
                        for fc in range(2):
                            for q in range(4):
                                dc = dh * 4 + q
                                MM(pys[q][:, 0:NT], w2v[:, fc, dc * 128:(dc + 1) * 128], hidT[:, fc, 0:NT], fc == 0, fc == 1,
                                   [sl2, hidTb[fc]], [pys[q]])
                        for q in range(4):
                            dc = dh * 4 + q
                            if ex == 0:
                                ACT(FM[:, dc, 0:NT], pys[q][:, 0:NT], AF.Copy, [pys[q]], [FM])
                            else:
                                TTo("dve", FM[:, dc, 0:NT], FM[:, dc, 0:NT], pys[q][:, 0:NT], ALU.add, [FM, pys[q]], [FM])
                DMA("sp", lnr[0][:], ln_rows[l, 2].partition_broadcast(128), [], [lnr[0]])
                DMA("sp", lnr[1][:], ln_rows[l, 3].partition_broadcast(128), [], [lnr[1]])
                for dc in range(8):
                    gate_evac(kind, l, 5, dc, FM[:, dc, 0:NT], FM[:, dc, 0:NT], NT, [FM], [FM])
                for g in range(G):
                    pbs = [PS(), PS()]
                    for dc in range(8):
                        TR(pbs[dc // 4][:, (dc % 4) * 128:(dc % 4 + 1) * 128], FM[:, dc, g * 128:(g + 1) * 128], ID, [FM, cst], [pbs[dc // 4]])
                    layer_norm_tile(g, pbs, lnr[0], lnr[1])
                if stop_after == ("moe", l):
                    break

            for g in range(G):
                yb = Buf("y%d" % tiles[g])
                DMA("sp", y_all[tiles[g]], X[g][:], [X[g]], [yb])
                outs.append(yb)

        d = S.dq["sp"]
        fin = []
        for i, sem in enumerate(d["sems"]):
            if d["cnt"][i] > 0:
                fin.append((sem, d["cnt"][i] * 16))
        S.lists["sp"].append((fin, None, None, 0))
        with nc.Block() as block:
            S.emit(block)
    return nc, S


_CACHE = {}


def prep_inputs(inp):
    f = lambda a: np.ascontiguousarray(np.asarray(a, dtype=np.float32))
    shared = {}
    shared["consts_p"] = make_consts("p")
    shared["consts_s"] = make_consts("s")
    shared["trig"] = make_trig()
    shared["w_ada"] = f(inp["w_ada"])
    shared["b_adaT"] = f(np.asarray(inp["b_ada"]).reshape(2, 48, 128).transpose(2, 0, 1))
    shared["w_in"] = f(inp["w_in"])
    shared["w_gate"] = f(np.asarray(inp["gla_w_gate"]).transpose(1, 0, 2))
    shared["b_gateT"] = f(np.asarray(inp["gla_b_gate"]).T)
    shared["rows"] = f(np.concatenate([np.asarray(inp[k]) for k in
                                       ("gla_norm", "ret_norm", "ssd_norm", "ssd_dt_bias", "ssd_a_log", "ssd_d")], axis=1)[:, None, :])
    shared["conv_wT"] = f(np.asarray(inp["ssd_conv_w"]).reshape(2, 4, 6, 128).transpose(3, 0, 2, 1))
    shared["conv_bT"] = f(np.asarray(inp["ssd_conv_b"]).reshape(2, 6, 128).transpose(2, 0, 1))
    shared["w_out"] = f(inp["w_out"])
    shared["ln_rows"] = f(np.stack([np.asarray(inp[k]) for k in ("ln1_g", "ln1_b", "ln2_g", "ln2_b")], axis=1)[:, :, None, :])
    wr = np.concatenate([np.asarray(inp["moe_w_group"]), np.asarray(inp["moe_w_expert"])], axis=2)
    shared["w_rt"] = f(wr.reshape(2, 8, 128, 20).transpose(2, 0, 1, 3))
    shared["b_rt"] = f(np.concatenate([np.asarray(inp["moe_b_group"]), np.asarray(inp["moe_b_expert"])], axis=1)[:, None, :])
    shared["moe_w1"] = f(inp["moe_w1"])
    shared["moe_w3"] = f(inp["moe_w3"])
    shared["moe_w2"] = f(inp["moe_w2"])
    xp = np.asarray(inp["x_prompt"], dtype=np.float32)
    xs = np.asarray(inp["x_sample"], dtype=np.float32)
    cp = np.asarray(inp["c_prompt"], dtype=np.float32)
    cs = np.asarray(inp["c_sample"], dtype=np.float32)
    maps = []
    for c in range(NCORE):
        m = dict(shared)
        m["x_all"] = f(np.concatenate([xp[c].reshape(16, 128, D), xs[16 * c:16 * c + 16].reshape(1, 128, D)], axis=0))
        cc = np.zeros((18, D), np.float32)
        cc[0] = cp[c]
        cc[1:17] = cs[16 * c:16 * c + 16]
        m["cT"] = f(cc.reshape(18, 8, 128).transpose(2, 1, 0))
        m["st_gla"] = f(np.asarray(inp["state_gla"])[:, 16 * c:16 * c + 16])
        m["st_ret"] = f(np.asarray(inp["state_ret"])[:, 16 * c:16 * c + 16])
        m["st_ssd"] = f(np.asarray(inp["state_ssd"])[:, 16 * c:16 * c + 16])
        sc = np.asarray(inp["state_conv"])[:, 16 * c:16 * c + 16]
        m["st_conv"] = f(sc.reshape(2, 16, 3, 6, 128).transpose(0, 4, 3, 1, 2))
        maps.append(m)
    return maps


def assemble(results):
    y_prompt = np.zeros((8, 2048, D), np.float32)
    y_sample = np.zeros((128, 8, D), np.float32)
    gla_p = np.zeros((2, 8, 4, 32, 64), np.float32)
    ret_p = np.zeros((2, 8, 4, 64, 64), np.float32)
    ssd_p = np.zeros((2, 8, 8, 64, 64), np.float32)
    conv_p = np.zeros((2, 8, 3, 768), np.float32)
    gla_s = np.zeros((2, 128, 4, 32, 64), np.float32)
    ret_s = np.zeros((2, 128, 4, 64, 64), np.float32)
    ssd_s = np.zeros((2, 128, 8, 64, 64), np.float32)
    conv_s = np.zeros((2, 128, 3, 768), np.float32)
    for c, r in enumerate(results):
        y = r["y_all"]
        y_prompt[c] = y[:16].reshape(2048, D)
        y_sample[16 * c:16 * c + 16] = y[16].reshape(16, 8, D)
        gla_p[:, c] = r["o_gla_p"].reshape(2, 4, 32, 64)
        ret_p[:, c] = r["o_ret_p"].reshape(2, 2, 2, 64, 64).reshape(2, 4, 64, 64)
        ssd_p[:, c] = r["o_ssd_p"].reshape(2, 4, 2, 64, 64).reshape(2, 8, 64, 64)
        conv_p[:, c] = r["o_conv_p"]
        gla_s[:, 16 * c:16 * c + 16] = r["o_gla_s"].reshape(2, 4, 32, 16, 64).transpose(0, 3, 1, 2, 4)
        ret_s[:, 16 * c:16 * c + 16] = r["o_ret_s"].reshape(2, 2, 2, 64, 16, 64).transpose(0, 4, 1, 2, 3, 5).reshape(2, 16, 4, 64, 64)
        ssd_s[:, 16 * c:16 * c + 16] = r["o_ssd_s"].reshape(2, 4, 2, 64, 16, 64).transpose(0, 4, 1, 2, 3, 5).reshape(2, 16, 8, 64, 64)
        conv_s[:, 16 * c:16 * c + 16] = r["o_conv_s"].reshape(2, 16, 3, 768)
    return (y_prompt, y_sample, gla_p, ret_p, ssd_p, conv_p, gla_s, ret_s, ssd_s, conv_s)


def kernel(**inputs):
    if "nc" not in _CACHE:
        _CACHE["nc"] = build()[0]
    nc = _CACHE["nc"]
    maps = prep_inputs(inputs)
    res = run_bass_kernel_spmd(nc, maps, core_ids=list(range(NCORE)))
    return assemble(res.results)
```
